# Optimizing a Trainium2 kernel written in Bass

```python
import math
import jax, jax.numpy as jnp
from jax import lax
import numpy as np

D_MODEL = 1024
BATCH = 4
SEQ = 8192
DEPTH = 1

HG_HEADS = 8
HG_DK = 128
HG_DV = D_MODEL // HG_HEADS
HG_CHUNK = 32

NSA_HEADS = 8
NSA_GROUPS = 2
NSA_REP = NSA_HEADS // NSA_GROUPS
NSA_DK = 128
NSA_DV = 128
CMP_BLOCK = 32
CMP_STRIDE = 16
CMP_HIDDEN = 128
SLC_BLOCK = 64
SLC_TOPK = 16
WINDOW = 512
Q_BLOCK = 64
FORCE_SCORE = 1e4

REL_BUCKETS = 32
REL_MAX_DIST = 128

N_GROUPS = 4
EXP_PER_GROUP = 8
N_EXPERTS = N_GROUPS * EXP_PER_GROUP
TOPK_IN_GROUP = 2
D_EXPERT = D_MODEL // 2
MOE_BLOCK = 128

ALPHA = (2 * DEPTH) ** 0.25
BETA = (8 * DEPTH) ** -0.25

SPLITS = (HG_HEADS * HG_DK, HG_HEADS * HG_DK, HG_HEADS * HG_DV, HG_HEADS * HG_DV,
          NSA_HEADS * NSA_DK,
          NSA_GROUPS * NSA_DK, NSA_GROUPS * NSA_DV,
          NSA_GROUPS * NSA_DK, NSA_GROUPS * NSA_DV,
          NSA_GROUPS * NSA_DK, NSA_GROUPS * NSA_DV,
          3 * NSA_HEADS, D_MODEL, D_MODEL)
D_IN = sum(SPLITS)

kernel_name = 'hybrid_hgrn2_nsa_hmoe_block'


def _layer_norm(x, eps=1e-5):
    xf = x.astype(jnp.float32)
    mu = jnp.mean(xf, axis=-1, keepdims=True)
    var = jnp.mean(jnp.square(xf - mu), axis=-1, keepdims=True)
    return ((xf - mu) * lax.rsqrt(var + eps)).astype(x.dtype)


def _rms_norm(x, eps=1e-6):
    xf = x.astype(jnp.float32)
    return (xf * lax.rsqrt(jnp.mean(jnp.square(xf), axis=-1, keepdims=True) + eps)).astype(x.dtype)


def _masked_softmax(logits, mask):
    lf = jnp.where(mask, logits.astype(jnp.float32), -1e30)
    p = jax.nn.softmax(lf, axis=-1)
    return jnp.where(mask, p, 0.0)


def _rel_bucket(dist):
    n = jnp.maximum(dist, 0)
    max_exact = REL_BUCKETS // 2
    nf = jnp.maximum(n, 1).astype(jnp.float32)
    large = max_exact + (jnp.log(nf / max_exact) / math.log(REL_MAX_DIST / max_exact)
                         * (REL_BUCKETS - max_exact)).astype(jnp.int32)
    large = jnp.minimum(large, REL_BUCKETS - 1)
    return jnp.where(n < max_exact, n, large)


def _hgrn2(q, f_raw, v, lb):
    B, T = q.shape[:2]
    N = T // HG_CHUNK
    lb = lb.reshape(HG_HEADS, HG_DK).astype(q.dtype)
    f = lb + (1 - lb) * jax.nn.sigmoid(f_raw)
    log_f = jnp.log(f)
    k = 1 - f

    def chunks(a):
        return a.reshape(B, N, HG_CHUNK, HG_HEADS, a.shape[-1]).transpose(1, 0, 3, 2, 4)

    qc, kc, vc, lfc = chunks(q), chunks(k), chunks(v), chunks(log_f)
    b = jnp.cumsum(lfc, axis=3)
    b_last = b[:, :, :, -1:, :]
    q_in = qc * jnp.exp(b)
    k_in = kc * jnp.exp(-b)
    k_end = kc * jnp.exp(b_last - b)
    causal = jnp.tril(jnp.ones((HG_CHUNK, HG_CHUNK), dtype=bool))
    att = jnp.einsum('nbhid,nbhjd->nbhij', q_in, k_in)
    att = jnp.where(causal, att, 0)
    o_intra = jnp.einsum('nbhij,nbhje->nbhie', att, vc)
    upd = jnp.einsum('nbhcd,nbhce->nbhde', k_end, vc)
    decay = jnp.exp(b_last[:, :, :, 0, :])

    def step(S, xs):
        q_n, dec_n, u_n = xs
        o_n = jnp.einsum('bhcd,bhde->bhce', q_n, S)
        S = dec_n[..., None] * S + u_n
        return S, o_n

    S0 = jnp.zeros((B, HG_HEADS, HG_DK, HG_DV), q.dtype)
    _, o_inter = lax.scan(step, S0, (q_in, decay, upd))
    o = o_intra + o_inter
    return o.transpose(1, 0, 3, 2, 4).reshape(B, T, HG_HEADS, HG_DV)


def _compress(kv, pos, w1, b1, w2):
    B, G, T, d = kv.shape
    r = CMP_BLOCK // CMP_STRIDE
    ns = T // CMP_STRIDE
    sub = kv.reshape(B, G, ns, CMP_STRIDE, d)
    blocks = jnp.concatenate([sub[:, :, j:ns - r + 1 + j] for j in range(r)], axis=3)
    blocks = (blocks + pos).reshape(B, G, ns - r + 1, CMP_BLOCK * d)
    return jax.nn.silu(blocks @ w1 + b1) @ w2


def _nsa(q, kc, vc, ks, vs, kw, vw, gate_raw, rel_bias,
         pos_k, w1_k, b1_k, w2_k, pos_v, w1_v, b1_v, w2_v):
    B, T = q.shape[:2]
    G, R = NSA_GROUPS, NSA_REP
    NQB = T // Q_BLOCK
    NSLC = T // SLC_BLOCK
    NSEL = min(SLC_TOPK, NSLC)
    scale = NSA_DK ** -0.5
    to_g = lambda a: a.transpose(0, 2, 1, 3)

    k_cmp = _compress(to_g(kc), pos_k, w1_k, b1_k, w2_k)
    v_cmp = _compress(to_g(vc), pos_v, w1_v, b1_v, w2_v)
    NC = k_cmp.shape[2]
    cmp_end = jnp.arange(NC) * CMP_STRIDE + CMP_BLOCK - 1
    cs = np.arange(NC) * CMP_STRIDE
    ss = np.arange(NSLC) * SLC_BLOCK
    overlap = jnp.asarray(((cs[:, None] < ss[None, :] + SLC_BLOCK)
                           & (cs[:, None] + CMP_BLOCK > ss[None, :])).astype(np.float32))

    k_slc = to_g(ks).reshape(B, G, NSLC, SLC_BLOCK, NSA_DK)
    v_slc = to_g(vs).reshape(B, G, NSLC, SLC_BLOCK, NSA_DV)
    pad = ((0, 0), (0, 0), (WINDOW, 0), (0, 0))
    k_win = jnp.pad(to_g(kw), pad)
    v_win = jnp.pad(to_g(vw), pad)
    table = rel_bias.reshape(G, R, REL_BUCKETS)
    g_ix = jnp.arange(G)[None, :, None, None, None]
    r_ix = jnp.arange(R)[None, None, :, None, None]
    blk_start = jnp.arange(NSLC) * SLC_BLOCK
    j_ix = jnp.arange(NSLC)

    q_blocks = q.reshape(B, NQB, Q_BLOCK, G, R, NSA_DK).transpose(1, 0, 3, 4, 2, 5)
    gate_blocks = jax.nn.sigmoid(gate_raw).reshape(B, NQB, Q_BLOCK, G, R, 3).transpose(1, 0, 3, 4, 2, 5)
    gather = jax.vmap(jax.vmap(lambda blk, ix: blk[ix]))

    def block_fn(args):
        qb, q_blk, g_blk = args
        t = qb * Q_BLOCK + jnp.arange(Q_BLOCK)
        dist_c = t[:, None] - cmp_end[None, :]
        s_c = jnp.einsum('bgrqd,bgnd->bgrqn', q_blk, k_cmp) * scale + table[:, :, _rel_bucket(dist_c)]
        p_c = _masked_softmax(s_c, dist_c >= 0)
        o_c = jnp.einsum('bgrqn,bgnd->bgrqd', p_c.astype(v_cmp.dtype), v_cmp)
        imp = jnp.einsum('bgqn,nj->bgqj', p_c.sum(axis=2), overlap)
        cur = t // SLC_BLOCK
        forced = (j_ix[None, :] == 0) | (j_ix[None, :] == cur[:, None]) | (j_ix[None, :] == cur[:, None] - 1)
        valid = blk_start[None, :] <= t[:, None]
        score = jnp.where(valid, jnp.where(forced, FORCE_SCORE, imp), -1.0)
        _, idx = lax.top_k(score, NSEL)
        k_sel = gather(k_slc, idx).reshape(B, G, Q_BLOCK, NSEL * SLC_BLOCK, NSA_DK)
        v_sel = gather(v_slc, idx).reshape(B, G, Q_BLOCK, NSEL * SLC_BLOCK, NSA_DV)
        s_pos = (idx[..., None] * SLC_BLOCK + jnp.arange(SLC_BLOCK)).reshape(B, G, Q_BLOCK, NSEL * SLC_BLOCK)
        dist_s = t[None, None, :, None] - s_pos
        s_s = (jnp.einsum('bgrqd,bgqkd->bgrqk', q_blk, k_sel) * scale
               + table[g_ix, r_ix, _rel_bucket(dist_s)[:, :, None]])
        p_s = _masked_softmax(s_s, (dist_s >= 0)[:, :, None])
        o_s = jnp.einsum('bgrqk,bgqkd->bgrqd', p_s.astype(v_sel.dtype), v_sel)
        k_w = lax.dynamic_slice_in_dim(k_win, qb * Q_BLOCK, WINDOW + Q_BLOCK, axis=2)
        v_w = lax.dynamic_slice_in_dim(v_win, qb * Q_BLOCK, WINDOW + Q_BLOCK, axis=2)
        pos_w = qb * Q_BLOCK - WINDOW + jnp.arange(WINDOW + Q_BLOCK)
        dist_w = t[:, None] - pos_w[None, :]
        mask_w = (dist_w >= 0) & (dist_w < WINDOW) & (pos_w[None, :] >= 0)
        s_w = jnp.einsum('bgrqd,bgkd->bgrqk', q_blk, k_w) * scale + table[:, :, _rel_bucket(dist_w)]
        p_w = _masked_softmax(s_w, mask_w)
        o_w = jnp.einsum('bgrqk,bgkd->bgrqd', p_w.astype(v_w.dtype), v_w)
        return g_blk[..., 0:1] * o_c + g_blk[..., 1:2] * o_s + g_blk[..., 2:3] * o_w

    o = lax.map(block_fn, (jnp.arange(NQB), q_blocks, gate_blocks))
    return o.transpose(1, 0, 4, 2, 3, 5).reshape(B, T, NSA_HEADS * NSA_DV)


def _hier_moe(h, w_grp, b_grp, w_exp, b_exp, w1, w3, w2):
    B, T, D = h.shape
    x = h.reshape(-1, D)
    NT = x.shape[0]
    grp_prob = jax.nn.softmax((x @ w_grp + b_grp).astype(jnp.float32), axis=-1)
    grp_p, grp_idx = lax.top_k(grp_prob, 1)
    exp_logits = (x @ w_exp + b_exp).astype(jnp.float32).reshape(NT, N_GROUPS, EXP_PER_GROUP)
    in_grp = exp_logits[jnp.arange(NT), grp_idx[:, 0]]
    top_l, top_i = lax.top_k(in_grp, TOPK_IN_GROUP)
    comb = grp_p * jax.nn.softmax(top_l, axis=-1)
    expert = grp_idx * EXP_PER_GROUP + top_i

    A = NT * TOPK_IN_GROUP
    e_flat = expert.reshape(-1)
    tok_flat = jnp.repeat(jnp.arange(NT), TOPK_IN_GROUP)
    order = jnp.argsort(e_flat)
    e_s, tok_s, w_s = e_flat[order], tok_flat[order], comb.reshape(-1)[order]
    counts = jnp.bincount(e_flat, length=N_EXPERTS)
    starts = jnp.cumsum(counts) - counts
    padded = (counts + MOE_BLOCK - 1) // MOE_BLOCK * MOE_BLOCK
    pend = jnp.cumsum(padded)
    pstarts = pend - padded
    dest = pstarts[e_s] + jnp.arange(A) - starts[e_s]
    n_blocks = -(-A // MOE_BLOCK) + N_EXPERTS
    x_pad = jnp.zeros((n_blocks * MOE_BLOCK, D), x.dtype).at[dest].set(x[tok_s])
    block_expert = jnp.minimum(jnp.searchsorted(pend, jnp.arange(n_blocks) * MOE_BLOCK, side='right'),
                               N_EXPERTS - 1)

    def expert_block(args):
        xb, e = args
        return (jax.nn.silu(xb @ w1[e]) * (xb @ w3[e])) @ w2[e]

    y_pad = lax.map(expert_block, (x_pad.reshape(n_blocks, MOE_BLOCK, D), block_expert)).reshape(-1, D)
    y = y_pad[dest] * w_s[:, None].astype(x.dtype)
    out = jnp.zeros_like(x).at[tok_s].add(y)
    return out.reshape(B, T, D)


def setup_inputs(seed: int = 0) -> dict:
    key = jax.random.key(seed)
    ks = iter(jax.random.split(key, 40))
    nrm = lambda shape, s: jax.random.normal(next(ks), shape, jnp.float32) * s
    L, D = DEPTH, D_MODEL
    return {
        'x': nrm((BATCH, SEQ, D), 1.0),
        'c': nrm((BATCH, D), 1.0),
        'ada_w': nrm((L, D, 6 * D), 0.1 * D ** -0.5),
        'ada_b': nrm((L, 6 * D), 0.02),
        'w_in': nrm((L, D, D_IN), D ** -0.5),
        'b_in': nrm((L, D_IN), 0.02),
        'hg_lb_logits': nrm((DEPTH + 1, HG_HEADS * HG_DK), 0.1),
        'hg_norm_w': 1.0 + nrm((L, HG_HEADS * HG_DV), 0.05),
        'cmp_pos_k': nrm((L, CMP_BLOCK, NSA_DK), 0.1),
        'cmp_w1_k': nrm((L, CMP_BLOCK * NSA_DK, CMP_HIDDEN), (CMP_BLOCK * NSA_DK) ** -0.5),
        'cmp_b1_k': nrm((L, CMP_HIDDEN), 0.02),
        'cmp_w2_k': nrm((L, CMP_HIDDEN, NSA_DK), CMP_HIDDEN ** -0.5),
        'cmp_pos_v': nrm((L, CMP_BLOCK, NSA_DV), 0.1),
        'cmp_w1_v': nrm((L, CMP_BLOCK * NSA_DV, CMP_HIDDEN), (CMP_BLOCK * NSA_DV) ** -0.5),
        'cmp_b1_v': nrm((L, CMP_HIDDEN), 0.02),
        'cmp_w2_v': nrm((L, CMP_HIDDEN, NSA_DV), CMP_HIDDEN ** -0.5),
        'rel_bias': nrm((NSA_HEADS, REL_BUCKETS), 0.5),
        'w_br_hg': nrm((L, HG_HEADS * HG_DV, D), BETA * (HG_HEADS * HG_DV) ** -0.5),
        'w_br_nsa': nrm((L, NSA_HEADS * NSA_DV, D), BETA * (NSA_HEADS * NSA_DV) ** -0.5),
        'w_out': nrm((L, D, D), BETA * D ** -0.5),
        'ln1_g': 1.0 + nrm((L, D), 0.05),
        'ln1_b': nrm((L, D), 0.02),
        'router_grp_w': nrm((L, D, N_GROUPS), D ** -0.5),
        'router_grp_b': nrm((L, N_GROUPS), 0.01),
        'router_exp_w': nrm((L, D, N_EXPERTS), D ** -0.5),
        'router_exp_b': nrm((L, N_EXPERTS), 0.01),
        'exp_w1': nrm((L, N_EXPERTS, D, D_EXPERT), D ** -0.5),
        'exp_w3': nrm((L, N_EXPERTS, D, D_EXPERT), D ** -0.5),
        'exp_w2': nrm((L, N_EXPERTS, D_EXPERT, D), BETA * D_EXPERT ** -0.5),
        'ln2_g': 1.0 + nrm((L, D), 0.05),
        'ln2_b': nrm((L, D), 0.02),
    }


def reference(x, c, ada_w, ada_b, w_in, b_in, hg_lb_logits, hg_norm_w,
              cmp_pos_k, cmp_w1_k, cmp_b1_k, cmp_w2_k, cmp_pos_v, cmp_w1_v, cmp_b1_v, cmp_w2_v,
              rel_bias, w_br_hg, w_br_nsa, w_out, ln1_g, ln1_b,
              router_grp_w, router_grp_b, router_exp_w, router_exp_b,
              exp_w1, exp_w3, exp_w2, ln2_g, ln2_b):
    B, T, D = x.shape
    split_idx = [int(v) for v in np.cumsum(SPLITS)[:-1]]
    lb_all = jnp.cumsum(jax.nn.softmax(hg_lb_logits.astype(jnp.float32), axis=0), axis=0)
    c_act = jax.nn.silu(c)
    for l in range(DEPTH):
        mod = c_act @ ada_w[l] + ada_b[l]
        sh1, sc1, g1, sh2, sc2, g2 = jnp.split(mod, 6, axis=-1)

        h = _layer_norm(x) * (1 + sc1[:, None]) + sh1[:, None]
        proj = h @ w_in[l] + b_in[l]
        (hq, hf, hi, hg, nq, kc, vc, ks, vs, kw, vw, ngate, mg_h, mg_n) = jnp.split(proj, split_idx, axis=-1)
        o_h = _hgrn2(hq.reshape(B, T, HG_HEADS, HG_DK), hf.reshape(B, T, HG_HEADS, HG_DK),
                     hi.reshape(B, T, HG_HEADS, HG_DV), lb_all[l])
        o_h = (_rms_norm(o_h) * hg_norm_w[l].reshape(HG_HEADS, HG_DV)
               * jax.nn.sigmoid(hg.reshape(B, T, HG_HEADS, HG_DV))).reshape(B, T, HG_HEADS * HG_DV)
        gsz = (B, T, NSA_GROUPS, NSA_DK)
        o_n = _nsa(nq.reshape(B, T, NSA_HEADS, NSA_DK),
                   kc.reshape(gsz), vc.reshape(gsz), ks.reshape(gsz), vs.reshape(gsz),
                   kw.reshape(gsz), vw.reshape(gsz),
                   ngate.reshape(B, T, NSA_HEADS, 3), rel_bias,
                   cmp_pos_k[l], cmp_w1_k[l], cmp_b1_k[l], cmp_w2_k[l],
                   cmp_pos_v[l], cmp_w1_v[l], cmp_b1_v[l], cmp_w2_v[l])
        merged = jax.nn.sigmoid(mg_h) * (o_h @ w_br_hg[l]) + jax.nn.sigmoid(mg_n) * (o_n @ w_br_nsa[l])
        y = (1 + g1[:, None]) * (merged @ w_out[l])
        x = _layer_norm(ALPHA * x + y) * ln1_g[l] + ln1_b[l]

        h = _layer_norm(x) * (1 + sc2[:, None]) + sh2[:, None]
        y = (1 + g2[:, None]) * _hier_moe(h, router_grp_w[l], router_grp_b[l], router_exp_w[l],
                                          router_exp_b[l], exp_w1[l], exp_w3[l], exp_w2[l])
        x = _layer_norm(ALPHA * x + y) * ln2_g[l] + ln2_b[l]
    return x
```

```python
import numpy as np
import concourse.bass as bass
import concourse.mybir as mybir
from contextlib import ExitStack

F32 = mybir.dt.float32
BF16 = mybir.dt.bfloat16
I32 = mybir.dt.int32
U32 = mybir.dt.uint32
AF = mybir.ActivationFunctionType
ALU = mybir.AluOpType
AX = mybir.AxisListType

ENGS = ("pe", "act", "dve", "pool", "sp")
NDMA = 6


class Prog:
    def __init__(self, nc, same_engine_sync=True):
        self.nc = nc
        self.es = ExitStack()
        self.ops = {e: [] for e in ENGS}
        self.cnt = {e: 0 for e in ENGS}
        self.sem = {e: self.es.enter_context(nc.semaphore("s_" + e)) for e in ENGS}
        self.dsem = {}
        self.dcnt = {}
        self.dlast = {}
        self.drr = {}
        for e in ("sp", "pool", "act"):
            self.dsem[e] = [self.es.enter_context(nc.semaphore("d_%s%d" % (e, i))) for i in range(NDMA)]
            self.dcnt[e] = [0] * NDMA
            self.drr[e] = 0
        self.seen = {e: {} for e in ENGS}
        self.lastw = {}
        self.readers = {}
        self.same = same_engine_sync
        self.out_tokens = []
        self.n_sb = 0
        self.pfx = ""
        self.ts_ = ExitStack()

    def sb(self, name, shape, dt):
        return self.ts_.enter_context(self.nc.sbuf_tensor("sb_" + self.pfx + name, list(shape), dt))

    def ps(self, name, shape, dt):
        return self.ts_.enter_context(self.nc.psum_tensor("ps_" + self.pfx + name, list(shape), dt))

    def release_tensors(self):
        self.ts_.close()
        self.ts_ = ExitStack()

    def barrier(self):
        toks = []
        for e in ENGS:
            if self.cnt[e] > 0:
                toks.append(("s_" + e, self.sem[e], self.cnt[e]))
        for e in self.dsem:
            for j in range(NDMA):
                if self.dcnt[e][j] > 0:
                    toks.append(("d_%s%d" % (e, j), self.dsem[e][j], self.dcnt[e][j]))
        for e in ENGS:
            waits = []
            for (sname, sh, val) in toks:
                if sname == "s_" + e:
                    continue
                if self.seen[e].get(sname, 0) >= val:
                    continue
                waits.append((sh, val))
                self.seen[e][sname] = val
            self.ops[e].append((waits, None, None, 0))

    def _deps(self, eng, reads, writes, is_mm=False):
        toks = []
        for k in reads:
            w = self.lastw.get(k)
            if w is not None:
                toks.append(w)
        for k in writes:
            w = self.lastw.get(k)
            if w is not None:
                toks.append(w)
            for r in self.readers.get(k, {}).values():
                toks.append(r)
        need = {}
        for (sname, sh, val, teng) in toks:
            if teng == eng and sname.startswith("s_"):
                if not self.same or (is_mm and eng == "pe"):
                    continue
            if self.seen[eng].get(sname, 0) >= val:
                continue
            if need.get(sname, (None, 0))[1] < val:
                need[sname] = (sh, val)
        for sname, (sh, val) in need.items():
            self.seen[eng][sname] = val
        return list(need.values())

    def _record(self, tok, reads, writes):
        for k in writes:
            self.lastw[k] = tok
            self.readers[k] = {}
        for k in reads:
            d = self.readers.setdefault(k, {})
            key = tok[0]
            if key not in d or d[key][2] < tok[2]:
                d[key] = tok

    def op(self, eng, fn, reads=(), writes=(), is_mm=False):
        waits = self._deps(eng, reads, writes, is_mm)
        self.cnt[eng] += 1
        tok = ("s_" + eng, self.sem[eng], self.cnt[eng], eng)
        self.ops[eng].append((waits, fn, self.sem[eng], 1))
        self._record(tok, reads, writes)
        return tok

    def dma(self, eng, fn, reads=(), writes=(), is_out=False):
        j = self.drr[eng]
        self.drr[eng] = (j + 1) % NDMA
        sh = self.dsem[eng][j]
        sname = "d_%s%d" % (eng, j)
        waits = self._deps(eng, reads, writes)
        prev = self.dcnt[eng][j]
        if prev > 0 and self.seen[eng].get(sname, 0) < prev:
            waits.append((sh, prev))
            self.seen[eng][sname] = prev
        self.dcnt[eng][j] += 16
        tok = (sname, sh, self.dcnt[eng][j], "dma_" + eng)
        self.ops[eng].append((waits, fn, sh, 16))
        self._record(tok, reads, writes)
        if is_out:
            self.out_tokens.append(tok)
        return tok

    def finish(self):
        nc = self.nc
        final_waits = {}
        for (sname, sh, val, _) in self.out_tokens:
            if final_waits.get(sname, (None, 0))[1] < val:
                final_waits[sname] = (sh, val)
        fw = list(final_waits.values())
        engmap = {"pe": "tensor", "act": "scalar", "dve": "vector", "pool": "gpsimd", "sp": "sync"}
        with nc.Block() as block:
            for e in ENGS:
                ops = self.ops[e]
                extra = fw if e == "sp" else []

                def body(engine, ops=ops, extra=extra):
                    for (waits, fn, sh, inc) in ops:
                        for (wsh, wval) in waits:
                            engine.wait_ge(wsh, wval)
                        if fn is None:
                            continue
                        ins = fn(engine)
                        ins.then_inc(sh, inc)
                    for (wsh, wval) in extra:
                        engine.wait_ge(wsh, wval)

                getattr(block, engmap[e])(body)
        self.ts_.close()
        self.es.close()


import os
from concourse.bass_utils import run_bass_kernel_spmd

D = 1024
NEG = -30000.0
SCALE = 128 ** -0.5


class W:
    def __init__(self, P):
        self.P = P

    def mm(self, out, lhsT, rhs, start, stop, reads, writes):
        return self.P.op("pe", lambda e: e.matmul(out, lhsT, rhs, start=start, stop=stop), reads, writes, is_mm=True)

    def tr(self, out, in_, ident, reads, writes):
        return self.P.op("pe", lambda e: e.transpose(out, in_, ident), reads, writes, is_mm=True)

    def act(self, out, in_, func, reads, writes, bias=0.0, scale=1.0):
        return self.P.op("act", lambda e: e.activation(out=out, in_=in_, func=func, bias=bias, scale=scale), reads, writes)

    def ts(self, out, in0, s1, s2, op0, op1, reads, writes, eng="dve"):
        return self.P.op(eng, lambda e: e.tensor_scalar(out=out, in0=in0, scalar1=s1, scalar2=s2, op0=op0, op1=op1), reads, writes)

    def tt(self, out, in0, in1, op, reads, writes, eng="dve"):
        return self.P.op(eng, lambda e: e.tensor_tensor(out=out, in0=in0, in1=in1, op=op), reads, writes)

    def stt(self, out, in0, scalar, in1, op0, op1, reads, writes):
        return self.P.op("dve", lambda e: e.scalar_tensor_tensor(out=out, in0=in0, scalar=scalar, in1=in1, op0=op0, op1=op1), reads, writes)

    def cp(self, out, in_, reads, writes, eng="dve"):
        if eng == "act":
            return self.P.op("act", lambda e: e.copy(out=out, in_=in_), reads, writes)
        return self.P.op(eng, lambda e: e.tensor_copy(out=out, in_=in_), reads, writes)

    def memset(self, ap, val, writes, eng="pool"):
        return self.P.op(eng, lambda e: e.memset(ap, val), (), writes)

    def ld(self, out, in_, writes, eng="sp", reads=()):
        return self.P.dma(eng, lambda e: e.dma_start(out=out, in_=in_), reads, writes)

    def st(self, out, in_, reads, writes=(), is_out=False, eng="sp"):
        return self.P.dma(eng, lambda e: e.dma_start(out=out, in_=in_), reads, writes, is_out=is_out)


def rel_bucket_np(dist):
    n = np.maximum(dist, 0)
    nf = np.maximum(n, 1).astype(np.float32)
    large = 16 + (np.log(nf / np.float32(16)) / np.float32(np.log(128 / 16)) * np.float32(16)).astype(np.int32)
    large = np.minimum(large, 31)
    return np.where(n < 16, n, large)


def host_consts(T):
    c = {}
    c["ident"] = np.eye(128, dtype=np.float32)
    j = np.arange(128)
    c["triU"] = (j[:, None] <= j[None, :]).astype(np.float32)
    kk = np.arange(2048)
    c["H32"] = (np.arange(32)[:, None] == (kk[None, :] // 64) % 32).astype(np.float32)
    NCT = max(1, T // 2048)
    n = np.arange(NCT * 128)
    jj = np.arange(128)
    ov = ((16 * n[:, None] < 64 * jj[None, :] + 64) & (16 * n[:, None] + 32 > 64 * jj[None, :])).astype(np.float32)
    ov[n >= T // 16 - 1] = 0.0
    c["ovm"] = ov.reshape(NCT, 128, 128).transpose(1, 0, 2).copy()
    qi = np.arange(128)
    cq = (qi >= 64).astype(np.int64)
    rel = np.arange(256) - 128
    A = (rel[None, :] < cq[:, None] - 1).astype(np.float32)
    Bm = np.where(rel[None, :] > cq[:, None], -1.0,
                  np.where(rel[None, :] >= cq[:, None] - 1, 1e4, 0.0)).astype(np.float32)
    c["selA"] = A
    c["selB"] = Bm
    sr = np.zeros((12, 12, 128), np.float32)
    for r in range(12):
        sr[r, r, :] = 1.0
    c["selrow"] = sr.reshape(12, 12 * 128)
    ki = np.arange(128)[:, None]
    qq = np.arange(128)[None, :]
    md = np.zeros((4, 128, 128), np.float32)
    md[0] = np.where(qq - ki >= 0, 0.0, NEG)
    md[2] = np.where(512 + qq - ki < 512, 0.0, NEG)
    md[3] = NEG
    c["maskD"] = md
    ni = np.arange(128)[:, None]
    q5 = np.arange(512)[None, :]
    mc = np.zeros((5, 128, 512), np.float32)
    for o in range(5):
        mc[o] = np.where(512 * o + q5 - 16 * ni - 31 >= 0, 0.0, NEG)
    c["maskC"] = mc
    return c


def host_bias_tables(rel_bias_g):
    ki = np.arange(128)[:, None]
    qq = np.arange(128)[None, :]
    bd = np.zeros((4, 4, 128, 128), np.float32)
    for k, off in enumerate((0, 128, 512)):
        bk = rel_bucket_np(off + qq - ki)
        bd[:, k] = rel_bias_g[:, bk]
    bd[:, 3] = rel_bias_g[:, 31][:, None, None]
    ni = np.arange(128)[:, None]
    q5 = np.arange(512)[None, :]
    bc = np.zeros((4, 5, 128, 512), np.float32)
    for o in range(5):
        bk = rel_bucket_np(512 * o + q5 - 16 * ni - 31)
        bc[:, o] = rel_bias_g[:, bk]
    c31 = np.broadcast_to(rel_bias_g[:, 31][None, :], (128, 4)).copy()
    return bd, bc, c31


def host_inputs_p1(inp, b, g, T):
    S = np.cumsum([0, 1024, 1024, 1024, 1024, 1024, 256, 256, 256, 256, 256, 256, 24, 1024, 1024])
    w_in = inp["w_in"][0]
    b_in = inp["b_in"][0]
    hs = slice(512 * g, 512 * g + 512)
    gs = slice(128 * g, 128 * g + 128)
    colsF = np.concatenate([np.arange(S[0], S[1])[hs], np.arange(S[1], S[2])[hs], np.arange(S[3], S[4])[hs],
                            np.arange(S[4], S[5])[hs], np.arange(S[5], S[6])[gs], np.arange(S[6], S[7])[gs],
                            np.arange(S[7], S[8])[gs], np.arange(S[9], S[10])[gs],
                            np.arange(S[11], S[12])[12 * g:12 * g + 12]])
    colsT = np.concatenate([np.arange(S[2], S[3])[hs], np.arange(S[8], S[9])[gs], np.arange(S[10], S[11])[gs]])
    wF = np.zeros((1024, 2688), np.float32)
    wF[:, :2572] = w_in[:, colsF]
    bFv = np.zeros((2688,), np.float32)
    bFv[:2572] = b_in[colsF]
    d = {}
    d["x"] = np.ascontiguousarray(inp["x"][b, :T])
    d["cT"] = np.ascontiguousarray(inp["c"][b].reshape(8, 128).T)
    d["adaw"] = np.ascontiguousarray(inp["ada_w"][0][:, 0:2048])
    d["adab"] = np.ascontiguousarray(inp["ada_b"][0][0:2048].reshape(16, 128).T)
    d["wF"] = wF
    d["bF"] = np.ascontiguousarray(bFv.reshape(21, 128).T)
    d["wT"] = np.ascontiguousarray(w_in[:, colsT])
    d["bT"] = np.ascontiguousarray(b_in[colsT][None, :])
    lg = inp["hg_lb_logits"]
    d["lbl"] = np.ascontiguousarray(np.concatenate([lg[0, hs].reshape(4, 128).T, lg[1, hs].reshape(4, 128).T], axis=1))
    d["normw"] = np.ascontiguousarray(inp["hg_norm_w"][0][hs].reshape(4, 128).T)
    for s in ("k", "v"):
        d["pos" + s] = np.ascontiguousarray(inp["cmp_pos_" + s][0].T)
        d["w1" + s] = np.ascontiguousarray(inp["cmp_w1_" + s][0])
        d["b1" + s] = np.ascontiguousarray(inp["cmp_b1_" + s][0][:, None])
        d["w2" + s] = np.ascontiguousarray(inp["cmp_w2_" + s][0])
    bd, bc, c31 = host_bias_tables(inp["rel_bias"][4 * g:4 * g + 4])
    d["biasD"], d["biasC"], d["c31"] = bd, bc, c31
    d.update(host_consts(T))
    return d


PER_GROUP = ("wF", "bF", "wT", "bT", "lbl", "normw", "biasD", "biasC", "c31")


def emit_p1(nc, P, w, T, ohT_o, onT_o, stage=99):
    NB = T // 512
    NT = T // 128
    NCT = max(1, T // 2048)
    sb, ps = P.sb, P.ps
    _cache = {}

    def dbg(*a, **k):
        return None

    wFb = sb("wFb", [128, 8, 2688], BF16); wTb = sb("wTb", [128, 8, 768], BF16)
    bFs = sb("bFs", [128, 21], F32); bTb = sb("bTb", [1, 768], BF16)
    identb = sb("identb", [128, 128], BF16); triU = sb("triU", [128, 128], F32)
    onesb = sb("onesb", [128, 128], BF16); onesf = sb("onesf", [128, 128], F32)
    H32 = sb("H32", [32, 2048], BF16); ovm = sb("ovm", [128, NCT, 128], BF16)
    selA = sb("selA", [128, 256], F32); selB = sb("selB", [128, 256], F32); selrow = sb("selrow", [12, 12 * 128], F32)
    Dt = sb("Dt", [128, 4, 4, 128], BF16)
    D4t = sb("D4t", [128, 4, 128], BF16)
    c31 = sb("c31", [128, 4], F32); nc31 = sb("nc31", [128, 4], F32)
    stg = sb("stg", [128, 1024], F32)
    cbt = [sb("cbt%d" % i, [128, 512], BF16) for i in range(2)]
    mod1 = sb("mod1", [128, 16], F32); scp1 = sb("scp1", [128, 8], F32)
    silc = sb("silc", [128, 8], F32); bFq = sb("bFq", [128, 4], F32)
    oacc4 = sb("oacc4", [128, 4, 512], F32)
    lb = sb("lb", [128, 4], F32); oml = sb("oml", [128, 4], F32); lbt = sb("lbt", [128, 8], F32); nw = sb("nw", [128, 4], F32)
    cw1 = sb("cw1", [128, 32, 128], BF16)
    cw2 = {s: sb("cw2" + s, [128, 128], BF16) for s in "kv"}
    cpos = {s: sb("cpos" + s, [128, 1], F32) for s in "kv"}
    posb = sb("posb", [128, 32], BF16); b1s = sb("b1s", [128, 2], F32)
    xt = sb("xt", [128, D], F32); xn = sb("xn", [128, 4, D], BF16)
    stat = sb("stat", [128, 16], F32)
    hT = sb("hT", [128, 8, 512], BF16)
    qT = sb("qT", [128, 512], F32); sg = sb("sg", [128, 512], F32); gT = sb("gT", [128, 512], F32)
    t1 = sb("t1", [128, 512], F32); t2 = sb("t2", [128, 512], F32); t3 = sb("t3", [128, 512], F32); t4 = sb("t4", [128, 512], F32)
    qe = sb("qe", [128, 512], BF16); kin = sb("kin", [128, 512], BF16); kendT = sb("kendT", [128, 512], BF16)
    dec = sb("dec", [128, 4], F32)
    kend4 = sb("kend4", [128, 512], BF16); attm4 = sb("attm4", [128, 512], BF16)
    S32 = sb("S32", [128, 4, 128], F32); S16v = sb("S16v", [128, 4, 5, 128], BF16)
    vtk = sb("vtk", [128, 4, 512], BF16)
    oT = stg[:, 0:512]; osq = stg[:, 512:1024]; stg2 = t4; ohb = sb("ohb", [128, 512], BF16)
    nqT = sb("nqT", [128, 4, 512], BF16)
    cext = {s: sb("cext" + s, [128, 528], BF16) for s in "kv"}
    hidT = {s: sb("hidT" + s, [128, NCT * 128], BF16) for s in "kv"}
    kcmpT = sb("kcmpT", [128, NCT * 128], BF16); vcmp = sb("vcmp", [128, NCT, 128], BF16)
    ksT = sb("ksT", [128, T], BF16); vs = sb("vs", [128, NT, 128], BF16)
    kwT = sb("kwT", [128, 8, 128], BF16); vw = sb("vw", [128, 8, 128], BF16)
    gate = sb("gate", [12, 512], F32)
    PT = [sb("PT%d" % i, [128, 512], BF16) for i in range(3)]
    rZ = t1; fac = t2; impacc = t3
    onb = ohb
    rzt = sb("rzt", [128, 16], F32)
    sc = sb("sc", [128, 128], F32); scw = sb("scw", [128, 128], F32)
    m8 = sb("m8", [128, 16], F32); nsel = sb("nsel", [128, 128], BF16)
    nselT = sb("nselT", [32, 4, 512], BF16)
    B = [ps("B%d" % i, [128, 512], F32) for i in range(6)]
    TB = [ps("TB%d" % i, [128, 1024], BF16) for i in range(2)]

    for g in range(2):
        def din(name, shape, dt=F32):
            full = ("g%d_" % g + name) if name in PER_GROUP else name
            if full not in _cache:
                _cache[full] = nc.dram_tensor(full, list(shape), dt, kind="ExternalInput").ap()
            return _cache[full]

        x = din("x", [T, D]); cT = din("cT", [128, 8]); adaw = din("adaw", [D, 2048]); adab = din("adab", [128, 16])
        wF = din("wF", [D, 2688]); bF = din("bF", [128, 21]); wT = din("wT", [D, 768]); bT = din("bT", [1, 768])
        lbl = din("lbl", [128, 8]); normw = din("normw", [128, 4])
        cmpw = {}
        for s in ("k", "v"):
            cmpw[s] = (din("pos" + s, [128, 32]), din("w1" + s, [4096, 128]), din("b1" + s, [128, 1]), din("w2" + s, [128, 128]))
        biasD = din("biasD", [4, 4, 128, 128]); biasC = din("biasC", [4, 5, 128, 512]); c31d = din("c31", [128, 4])
        ident_d = din("ident", [128, 128]); triU_d = din("triU", [128, 128]); H32_d = din("H32", [32, 2048])
        ovm_d = din("ovm", [128, NCT, 128]); selA_d = din("selA", [128, 256]); selB_d = din("selB", [128, 256])
        selrow_d = din("selrow", [12, 12 * 128]); maskD_d = din("maskD", [4, 128, 128]); maskC_d = din("maskC", [5, 128, 512])
        w.memset(onesb[:], 1.0, ["onesb"]); w.memset(onesf[:], 1.0, ["onesf"])
        for (dst, src, nm) in ((identb, ident_d, "identb"), (H32, H32_d, "H32"), (ovm, ovm_d, "ovm"), (bTb, bT, "bTb")):
            w.ld(dst[:], src, [nm], eng="pool")
        for (dst, src, nm) in ((triU, triU_d, "triU"), (selA, selA_d, "selA"), (selB, selB_d, "selB"), (selrow, selrow_d, "selrow"),
                               (c31, c31d, "c31"), (bFs, bF, "bFs"), (lbt, lbl, "lbt"), (nw, normw, "nw"), (silc, cT, "silc"),
                               (mod1, adab, "adab_s")):
            w.ld(dst[:], src, [nm])
        for kc in range(8):
            w.ld(wFb[:, kc, :], wF[kc * 128:(kc + 1) * 128, :], ["wFb"], eng="pool")
            w.ld(wTb[:, kc, :], wT[kc * 128:(kc + 1) * 128, :], ["wTb"], eng="pool")
        w.ts(nc31[:], c31[:], -1.0, None, ALU.mult, ALU.bypass, ["c31"], ["nc31"])
        w.ts(bFq[:], bFs[:, 12:16], SCALE, None, ALU.mult, ALU.bypass, ["bFs"], ["bFq"])
        for h in range(4):
            for k in range(4):
                w.ld(stg[:, 0:128], biasD[h, k], ["stg"])
                w.ld(stg[:, 128:256], maskD_d[k], ["stg"])
                w.stt(Dt[:, h, k, :], stg[:, 0:128], c31[:, h:h + 1], stg[:, 128:256], ALU.subtract, ALU.add, ["stg", "c31"], ["Dt"])
                if k == 2:
                    w.stt(D4t[:, h, :], stg[:, 0:128], c31[:, h:h + 1], stg[:, 128:256], ALU.subtract, ALU.add, ["stg", "c31"], ["Dt"])
        w.tt(lb[:], lbt[:, 0:4], lbt[:, 4:8], ALU.subtract, ["lbt"], ["lb"])
        w.act(lb[:], lb[:], AF.Sigmoid, ["lb"], ["lb"])
        w.ts(oml[:], lb[:], -1.0, 1.0, ALU.mult, ALU.add, ["lb"], ["oml"])
        w.act(silc[:], silc[:], AF.Silu, ["silc"], ["silc"])
        for m in range(16):
            w.ld(stg[:, 0:1024].rearrange("p (k c) -> p k c", k=8), adaw[:, m * 128:(m + 1) * 128].rearrange("(k p) c -> p k c", p=128), ["stg"])
            for kc in range(8):
                w.mm(B[0][:, m:m + 1], stg[:, kc * 128:(kc + 1) * 128], silc[:, kc:kc + 1], kc == 0, kc == 7, ["stg", "silc"], ["B0"])
        w.tt(mod1[:], mod1[:], B[0][:, 0:16], ALU.add, ["adab_s", "B0"], ["mod1"])
        w.ts(scp1[:], mod1[:, 8:16], 1.0, None, ALU.add, ALU.bypass, ["mod1"], ["scp1"])
        for si, s in enumerate("kv"):
            posd, w1d, b1d, w2d = cmpw[s]
            w.ld(posb[:], posd, ["posb"], eng="pool")
            w.ld(cw1[:], w1d.rearrange("(r d) m -> d r m", d=128), ["cw1"], eng="pool")
            w.ld(cw2[s][:], w2d, ["cw2" + s], eng="pool")
            w.ld(b1s[:, si:si + 1], b1d, ["b1s"])
            for r in range(32):
                w.mm(B[1][:, si:si + 1], cw1[:, r, :], posb[:, r:r + 1], r == 0, r == 31, ["cw1", "posb"], ["B1"])
            w.tt(cpos[s][:], B[1][:, si:si + 1], b1s[:, si:si + 1], ALU.add, ["B1", "b1s"], ["cpos" + s])
            w.memset(cext[s][:], 0.0, ["cext" + s]); w.memset(hidT[s][:], 0.0, ["hidT" + s])
        w.memset(kcmpT[:], 0.0, ["kcmpT"]); w.memset(vcmp[:], 0.0, ["vcmp"])
        w.memset(S32[:], 0.0, ["S32"]); w.memset(S16v[:], 0.0, ["S16v0", "S16v1", "S16v2", "S16v3"])

        def load_cw1(s):
            w.ld(cw1[:], cmpw[s][1].rearrange("(r d) m -> d r m", d=128), ["cw1"], eng="pool")

        for I in range(NB if stage >= 1 else 0):
            t0 = I * 512
            for s4 in range(4):
                w.ld(xt[:], x[t0 + s4 * 128:t0 + (s4 + 1) * 128, :], ["xt"])
                w_ = P.op("dve", lambda e: e.bn_stats(out=stat[:, 0:6], in_=xt[:, 0:512]), ["xt"], ["stat"])
                P.op("dve", lambda e: e.bn_stats(out=stat[:, 6:12], in_=xt[:, 512:1024]), ["xt"], ["stat"])
                P.op("dve", lambda e: e.bn_aggr(out=stat[:, 12:14], in_=stat[:, 0:12].rearrange("p (a b) -> p a b", a=2)), ["stat"], ["stat"])
                w.act(stat[:, 14:15], stat[:, 13:14], AF.Ln, ["stat"], ["stat"], bias=1e-5)
                w.act(stat[:, 14:15], stat[:, 14:15], AF.Exp, ["stat"], ["stat"], scale=-0.5)
                w.stt(stat[:, 15:16], stat[:, 12:13], -1.0, stat[:, 14:15], ALU.mult, ALU.mult, ["stat"], ["stat"])
                w.act(xn[:, s4, :], xt[:], AF.Identity, ["xt", "stat"], ["xn"], bias=stat[:, 15:16], scale=stat[:, 14:15])
            for c in range(8):
                tb = TB[c % 2]
                for s4 in range(4):
                    w.tr(tb[:, s4 * 128:(s4 + 1) * 128], xn[:, s4, c * 128:(c + 1) * 128], identb[:], ["xn", "identb"], ["TB%d" % (c % 2)])
                w.ts(hT[:, c, :], tb[:, 0:512], scp1[:, c:c + 1], mod1[:, c:c + 1], ALU.mult, ALU.add, ["TB%d" % (c % 2), "scp1", "mod1"], ["hT"])

            dbg("hT", hT[:], [128, 8, 512], BF16, ["hT"])
            dbg("mod1", mod1[:], [128, 16], F32, ["mod1"])

            def projF(m, bank):
                rows = 12 if m == 20 else 128
                for kc in range(8):
                    w.mm(B[bank][0:rows, :], wFb[:, kc, m * 128:m * 128 + rows], hT[:, kc, :], kc == 0, kc == 7, ["wFb", "hT"], ["B%d" % bank])
                return rows

            for s4 in range(4):
                j = I * 4 + s4
                bk = s4 % 2
                for kc in range(8):
                    w.mm(B[bk][:, :], hT[:, kc, s4 * 128:(s4 + 1) * 128], wTb[:, kc, 0:512], kc == 0, False, ["hT", "wTb"], ["B%d" % bk])
                w.mm(B[bk][:, :], onesb[0:1, :], bTb[0:1, 0:512], False, True, ["onesb", "bTb"], ["B%d" % bk])
                w.cp(vtk[:, s4, :], B[bk][:, :], ["B%d" % bk], ["vtk"], eng="act")
                bk2 = 2 + s4 % 2
                for kc in range(8):
                    w.mm(B[bk2][:, 0:256], hT[:, kc, s4 * 128:(s4 + 1) * 128], wTb[:, kc, 512:768], kc == 0, False, ["hT", "wTb"], ["B%d" % bk2])
                w.mm(B[bk2][:, 0:256], onesb[0:1, :], bTb[0:1, 512:768], False, True, ["onesb", "bTb"], ["B%d" % bk2])
                w.cp(vs[:, j, :], B[bk2][:, 0:128], ["B%d" % bk2], ["vs"], eng="act")
                w.cp(vw[:, j % 8, :], B[bk2][:, 128:256], ["B%d" % bk2], ["vw"], eng="act")

            for h in range(4):
                projF(12 + h, h)
                w.act(nqT[:, h, :], B[h][:, :], AF.Identity, ["B%d" % h, "bFq"], ["nqT"], bias=bFq[:, h:h + 1], scale=SCALE)
            for (m, s, bkk) in ((16, "k", 4), (17, "v", 5)):
                projF(m, bkk)
                w.cp(cext[s][:, 0:16], cext[s][:, 512:528], ["cext" + s], ["cext" + s], eng="pool")
                w.act(cext[s][:, 16:528], B[bkk][:, :], AF.Identity, ["B%d" % bkk, "bFs"], ["cext" + s], bias=bFs[:, m:m + 1])
            projF(18, 0)
            w.act(ksT[:, t0:t0 + 512], B[0][:, :], AF.Identity, ["B0", "bFs"], ["ksT"], bias=bFs[:, 18:19])
            projF(19, 1)
            for s4 in range(4):
                w.act(kwT[:, (I * 4 + s4) % 8, :], B[1][:, s4 * 128:(s4 + 1) * 128], AF.Identity, ["B1", "bFs"], ["kwT"], bias=bFs[:, 19:20])
            projF(20, 2)
            w.act(gate[:, :], B[2][0:12, :], AF.Sigmoid, ["B2", "bFs"], ["gate"], bias=bFs[0:12, 20:21])

            for h in range(4 if stage >= 2 else 0):
                projF(h, 0)
                w.act(qT[:], B[0][:, :], AF.Identity, ["B0", "bFs"], ["qT"], bias=bFs[:, h:h + 1])
                projF(4 + h, 1)
                w.act(sg[:], B[1][:, :], AF.Sigmoid, ["B1", "bFs"], ["sg"], bias=bFs[:, 4 + h:5 + h])
                projF(8 + h, 5)
                w.act(gT[:], B[5][:, :], AF.Sigmoid, ["B5", "bFs"], ["gT"], bias=bFs[:, 8 + h:9 + h])
                w.ts(t1[:], sg[:], oml[:, h:h + 1], lb[:, h:h + 1], ALU.mult, ALU.add, ["sg", "oml", "lb"], ["t1"])
                w.act(t2[:], t1[:], AF.Ln, ["t1"], ["t2"])
                w.ts(t1[:], t1[:], -1.0, 1.0, ALU.mult, ALU.add, ["t1"], ["t1"])
                for s4 in range(4):
                    sl = slice(s4 * 128, (s4 + 1) * 128)
                    P.op("dve", lambda e, sl=sl: e.tensor_tensor_scan(out=t3[:, sl], data0=onesf[:, :], data1=t2[:, sl], initial=0.0,
                                                                      op0=ALU.mult, op1=ALU.add), ["t2", "onesf"], ["t3"])
                w.act(t2[:], t3[:], AF.Exp, ["t3"], ["t2"])
                w.act(t4[:], t3[:], AF.Exp, ["t3"], ["t4"], scale=-1.0)
                w.act(dec[:, :], t3[:, 127:512:128], AF.Exp, ["t3"], ["dec"])
                dbg("qT", qT[:], [128, 512], F32, ["qT"]); dbg("sg", sg[:], [128, 512], F32, ["sg"]); dbg("gT", gT[:], [128, 512], F32, ["gT"])
                dbg("b", t3[:], [128, 512], F32, ["t3"]); dbg("k", t1[:], [128, 512], F32, ["t1"]); dbg("vtk", vtk[:], [128, 4, 512], BF16, ["vtk"])
                dbg("dec", dec[:], [128, 4], F32, ["dec"])
                w.tt(qe[:], qT[:], t2[:], ALU.mult, ["qT", "t2"], ["qe"])
                w.tt(t4[:], t1[:], t4[:], ALU.mult, ["t1", "t4"], ["t4"])
                w.cp(kin[:], t4[:], ["t4"], ["kin"], eng="pool")
                for s4 in range(4):
                    sl = slice(s4 * 128, (s4 + 1) * 128)
                    w.ts(kendT[:, sl], t4[:, sl], dec[:, s4:s4 + 1], None, ALU.mult, ALU.bypass, ["t4", "dec"], ["kendT"])
                for s4 in range(4):
                    sl = slice(s4 * 128, (s4 + 1) * 128)
                    w.tr(TB[1][:, sl], kendT[:, sl], identb[:], ["kendT", "identb"], ["TB1"])
                w.cp(kend4[:], TB[1][:, 0:512], ["TB1"], ["kend4"], eng="act")
                for s4 in range(4):
                    sl = slice(s4 * 128, (s4 + 1) * 128)
                    w.mm(B[4][:, sl], kend4[:, sl], vtk[:, s4, h * 128:(h + 1) * 128], s4 == 0, s4 == 3, ["kend4", "vtk"], ["B4"])
                if I > 0:
                    w.cp(S16v[:, h, 0, :], S16v[:, h, 4, :], ["S16v%d" % h], ["S16v%d" % h], eng="act")
                for s4 in range(4):
                    sl = slice(s4 * 128, (s4 + 1) * 128)
                    w.stt(S32[:, h, :], S32[:, h, :], dec[:, s4:s4 + 1], B[4][:, sl], ALU.mult, ALU.add, ["S32", "dec", "B4"], ["S32"])
                    w.cp(S16v[:, h, s4 + 1, :], S32[:, h, :], ["S32"], ["S16v%d" % h], eng="act")
                for s4 in range(4):
                    sl = slice(s4 * 128, (s4 + 1) * 128)
                    w.mm(B[2][:, sl], kin[:, sl], qe[:, sl], s4 == 0, s4 == 3, ["kin", "qe"], ["B2"])
                for s4 in range(4):
                    sl = slice(s4 * 128, (s4 + 1) * 128)
                    w.tt(attm4[:, sl], B[2][:, sl], triU[:], ALU.mult, ["B2", "triU"], ["attm4"])
                for s4 in range(4):
                    sl = slice(s4 * 128, (s4 + 1) * 128)
                    w.mm(B[3][:, sl], vtk[:, s4, h * 128:(h + 1) * 128], attm4[:, sl], s4 == 0, False, ["vtk", "attm4"], ["B3"])
                    w.mm(B[3][:, sl], S16v[:, h, s4, :], qe[:, sl], False, s4 == 3, ["S16v%d" % h, "qe"], ["B3"])
                w.cp(oT[:], B[3][:, :], ["B3"], ["stg"], eng="act")
                dbg("stg", oT[:], [128, 512], F32, ["stg"])
                w.tt(osq[:], oT[:], oT[:], ALU.mult, ["stg"], ["stg"], eng="pool")
                w.mm(B[5][:, :], onesf[:], osq[:], True, True, ["onesf", "stg"], ["B5"])
                w.act(osq[:], B[5][:, :], AF.Ln, ["B5"], ["stg"], bias=1e-6, scale=1.0 / 128)
                w.act(osq[:], osq[:], AF.Exp, ["stg"], ["stg"], scale=-0.5)
                w.tt(oT[:], oT[:], osq[:], ALU.mult, ["stg", "stg"], ["stg"])
                w.stt(ohb[:], oT[:], nw[:, h:h + 1], gT[:], ALU.mult, ALU.mult, ["stg", "nw", "gT"], ["ohb"])
                w.st(ohT_o[g * 512 + h * 128:g * 512 + (h + 1) * 128, t0:t0 + 512], ohb[:], ["ohb"])

            if stage < 3:
                continue
            lo = 1 if I == 0 else 0
            c0, c1 = 32 * I - 1 + lo, 32 * I + 31
            for s in "kv":
                load_cw1(s)
                for r in range(32):
                    w.mm(B[0][:, 0:32], cw1[:, r, :], cext[s][:, r:r + 497:16], r == 0, r == 31, ["cw1", "cext" + s], ["B0"])
                w.act(hidT[s][:, c0:c1], B[0][:, lo:32], AF.Silu, ["B0", "cpos" + s], ["hidT" + s], bias=cpos[s][:, 0:1])
            w.mm(B[1][:, 0:c1 - c0], cw2["k"][:], hidT["k"][:, c0:c1], True, True, ["cw2k", "hidTk"], ["B1"])
            w.cp(kcmpT[:, c0:c1], B[1][:, 0:c1 - c0], ["B1"], ["kcmpT"], eng="act")
            for jt in sorted(set([max(c0, 0) // 128, (c1 - 1) // 128])):
                w.mm(B[1][:, 0:128], hidT["v"][:, jt * 128:(jt + 1) * 128], cw2["v"][:], True, True, ["hidTv", "cw2v"], ["B1"])
                w.cp(vcmp[:, jt, :], B[1][:, 0:128], ["B1"], ["vcmp"], eng="act")

            def run_units(h, units, want_U):
                nu = len(units)
                sbanks = [0, 1] if want_U else [0, 1, 4]
                nbk = len(sbanks)
                la = nbk - 1

                def emit_qk(u):
                    Kap, kreads, extras, Vap, vreads, ovap = units[u]
                    bk = sbanks[u % nbk]
                    bkey = "B%d" % bk
                    w.mm(B[bk][:, :], Kap, nqT[:, h, :], True, len(extras) == 0, kreads + ["nqT"], [bkey])
                    for ei, (cols, lT, rh, rds) in enumerate(extras):
                        w.mm(B[bk][:, cols], lT, rh, False, ei == len(extras) - 1, rds, [bkey])

                def emit_rest(u):
                    Kap, kreads, extras, Vap, vreads, ovap = units[u]
                    bk = sbanks[u % nbk]
                    bkey = "B%d" % bk
                    pt = PT[u % nbk]
                    pkey = "PT%d" % (u % nbk)
                    w.act(pt[:], B[bk][:, :], AF.Exp, [bkey, "c31"], [pkey], bias=c31[:, h:h + 1])
                    first, last = (u == 0), (u == nu - 1)
                    w.mm(B[2][:, :], Vap, pt[:], first, last, vreads + [pkey], ["B2"])
                    w.mm(B[3][:, :], onesb[:], pt[:], first, last, ["onesb", pkey], ["B3"])
                    if want_U:
                        for s4 in range(4):
                            sl = slice(s4 * 128, (s4 + 1) * 128)
                            w.mm(B[4][:, sl], pt[:, sl], ovap, first and s4 == 0, last and s4 == 3, [pkey, "ovm"], ["B4"])
                            w.mm(B[5][:, s4:s4 + 1], pt[:, sl], onesb[:, 0:1], first and s4 == 0, last and s4 == 3, [pkey, "onesb"], ["B5"])

                for u in range(min(la, nu)):
                    emit_qk(u)
                for u in range(nu):
                    if u + la < nu:
                        emit_qk(u + la)
                    emit_rest(u)

            def epilogue(h, br, first):
                w.ts(rZ[:], B[3][:, :], 1e-30, None, ALU.max, ALU.bypass, ["B3"], ["t1"])
                P.op("dve", lambda e: e.reciprocal(out=rZ[:], in_=rZ[:]), ["t1"], ["t1"])
                r = 3 * h + br
                w.mm(B[5][:, :], selrow[0:12, r * 128:(r + 1) * 128], gate[0:12, :], True, True, ["selrow", "gate"], ["B5"])
                w.tt(fac[:], rZ[:], B[5][:, :], ALU.mult, ["t1", "B5"], ["t2"])
                if first:
                    w.tt(oacc4[:, h, :], B[2][:, :], fac[:], ALU.mult, ["B2", "t2"], ["oacc%d" % h])
                else:
                    w.tt(rZ[:], B[2][:, :], fac[:], ALU.mult, ["B2", "t2"], ["t1"])
                    w.tt(oacc4[:, h, :], oacc4[:, h, :], rZ[:], ALU.add, ["oacc%d" % h, "t1"], ["oacc%d" % h], eng="pool")

            def diag_extras(h, j, window):
                ex = []
                for s4 in range(4):
                    o = 4 * I + s4 - j
                    if o < 0:
                        kind = 3
                    elif o == 0:
                        kind = 0
                    elif o == 1:
                        kind = 1
                    elif window and o == 4:
                        kind = int(os.environ.get("X_K2", "2"))
                    elif window and o >= 5:
                        kind = 3
                    else:
                        continue
                    ex.append((slice(s4 * 128, (s4 + 1) * 128), identb[:], D4t[:, h, :] if kind == 2 else Dt[:, h, kind, :], ["identb", "Dt"]))
                return ex

            for h in range(4 if stage >= 4 else 0):
                units = []
                for jt in range(I // 4 + 1):
                    o = I - 4 * jt
                    ex = []
                    if o <= 4:
                        w.ld(stg[:, 0:512], biasC[h, o], ["stg"])
                        w.ld(stg2[:], maskC_d[o], ["t4"])
                        w.stt(cbt[jt % 2][:], stg[:, 0:512], c31[:, h:h + 1], stg2[:], ALU.subtract, ALU.add, ["stg", "t4", "c31"], ["cbt%d" % (jt % 2)])
                        ex.append((slice(0, 512), identb[:], cbt[jt % 2][:], ["identb", "cbt%d" % (jt % 2)]))
                    units.append((kcmpT[:, jt * 128:(jt + 1) * 128], ["kcmpT"], ex, vcmp[:, jt, :], ["vcmp"], ovm[:, jt, :]))
                run_units(h, units, True)
                w.ts(rzt[:, 0:4], B[5][:, 0:4], 1e-30, None, ALU.max, ALU.bypass, ["B5"], ["rzt"])
                P.op("dve", lambda e: e.reciprocal(out=rzt[:, 0:4], in_=rzt[:, 0:4]), ["rzt"], ["rzt"])
                for s4 in range(4):
                    sl = slice(s4 * 128, (s4 + 1) * 128)
                    if h == 0:
                        w.ts(impacc[:, sl], B[4][:, sl], rzt[:, s4:s4 + 1], None, ALU.mult, ALU.bypass, ["B4", "rzt"], ["t3"])
                    else:
                        w.stt(impacc[:, sl], B[4][:, sl], rzt[:, s4:s4 + 1], impacc[:, sl], ALU.mult, ALU.add, ["B4", "rzt", "t3"], ["t3"])
                epilogue(h, 0, True)

            for s4 in range(4 if stage >= 5 else 0):
                tq = 4 * I + s4
                off = 128 - 2 * tq
                sl = slice(s4 * 128, (s4 + 1) * 128)
                w.tt(sc[:], impacc[:, sl], selA[:, off:off + 128], ALU.mult, ["t3", "selA"], ["sc"])
                w.tt(sc[:], sc[:], selB[:, off:off + 128], ALU.add, ["sc", "selB"], ["sc"])
                w.memset(sc[:, 0:1], 1e4, ["sc"], eng="dve")
                P.op("dve", lambda e: e.max(out=m8[:, 0:8], in_=sc[:]), ["sc"], ["m8"])
                P.op("dve", lambda e: e.match_replace(out=scw[:], in_to_replace=m8[:, 0:8], in_values=sc[:], imm_value=-1e30), ["sc", "m8"], ["scw"])
                P.op("dve", lambda e: e.max(out=m8[:, 8:16], in_=scw[:]), ["scw"], ["m8"])
                w.ts(scw[:], sc[:], m8[:, 15:16], None, ALU.is_ge, ALU.bypass, ["sc", "m8"], ["scw"])
                w.ts(nsel[:], scw[:], 32768.0, -32768.0, ALU.mult, ALU.add, ["scw"], ["nsel"])
                for g4 in range(4):
                    w.tr(TB[0][0:32, g4 * 128:(g4 + 1) * 128], nsel[:, g4 * 32:(g4 + 1) * 32], identb[:], ["nsel", "identb"], ["TB0"])
                w.cp(nselT[0:32, :, sl], TB[0][0:32, 0:512].rearrange("p (g q) -> p g q", g=4), ["TB0"], ["nselT"], eng="act")

            for h in range(4 if stage >= 6 else 0):
                units = []
                for j in range(max(0, 4 * I - 4), 4 * I + 4 if os.environ.get("X_WIN", "1") == "1" else 0):
                    units.append((kwT[:, j % 8, :], ["kwT"], diag_extras(h, j, True) if os.environ.get("X_WEX", "1") == "1" else [], vw[:, j % 8, :], ["vw"], None))
                if units:
                    run_units(h, units, False)
                    epilogue(h, 2, False)
            for h in range(4 if stage >= 6 else 0):
                units = []
                for j in range(4 * I + 4):
                    ex = []
                    if os.environ.get("X_MASK", "1") == "1":
                        ex = [(slice(0, 512), H32[0:32, 128 * (j % 16):128 * (j % 16) + 128], nselT[0:32, j // 16, :], ["H32", "nselT"])]
                    if j >= 4 * I - 1 and os.environ.get("X_DIAG", "1") == "1":
                        ex += diag_extras(h, j, False)
                    units.append((ksT[:, j * 128:(j + 1) * 128], ["ksT"], ex, vs[:, j, :], ["vs"], None))
                run_units(h, units, False)
                epilogue(h, 1, False)
                w.cp(onb[:], oacc4[:, h, :], ["oacc%d" % h], ["ohb"], eng="act")
                w.st(onT_o[g * 512 + h * 128:g * 512 + (h + 1) * 128, t0:t0 + 512], onb[:], ["ohb"])


ALPHA_C = float(2.0 ** 0.25)


def host_inputs_p2(inp, b, s, TT):
    d = {}
    tsl = slice(s * TT, (s + 1) * TT)
    d["x"] = np.ascontiguousarray(inp["x"][b, tsl])
    d["cT"] = np.ascontiguousarray(inp["c"][b].reshape(8, 128).T)
    d["adaw"] = inp["ada_w"][0]
    ab = inp["ada_b"][0]
    d["adabT"] = np.ascontiguousarray(ab.reshape(48, 128).T)
    d["adabB"] = np.ascontiguousarray(np.broadcast_to(ab[None, :], (128, 6144)))
    w_in = inp["w_in"][0]
    d["wG"] = np.ascontiguousarray(w_in[:, 6680:8728])
    d["bG"] = np.ascontiguousarray(inp["b_in"][0][6680:8728].reshape(16, 128).T)
    d["wbh"] = inp["w_br_hg"][0]; d["wbn"] = inp["w_br_nsa"][0]; d["wout"] = inp["w_out"][0]
    for nm in ("ln1_g", "ln1_b", "ln2_g", "ln2_b"):
        d[nm] = np.ascontiguousarray(np.broadcast_to(inp[nm][0][None, :], (128, 1024)))
    d["wr"] = np.ascontiguousarray(np.concatenate([inp["router_grp_w"][0], inp["router_exp_w"][0]], axis=1))
    d["br"] = np.ascontiguousarray(np.broadcast_to(np.concatenate([inp["router_grp_b"][0], inp["router_exp_b"][0]])[None, :], (128, 36)))
    d["ew1"] = inp["exp_w1"][0]; d["ew3"] = inp["exp_w3"][0]; d["ew2"] = inp["exp_w2"][0]
    d["ident"] = np.eye(128, dtype=np.float32)
    return d


def emit_p2(nc, P, w, TT, ohT, onT, n_exp=32):
    NB = TT // 512
    def din(name, shape, dt=F32):
        return nc.dram_tensor("q_" + name, list(shape), dt, kind="ExternalInput").ap()

    x = din("x", [TT, D]); hsel_d = din("hsel", [128, 2])
    cT = din("cT", [128, 8]); adaw = din("adaw", [D, 6144]); adabT = din("adabT", [128, 48]); adabB = din("adabB", [128, 6144])
    wG = din("wG", [D, 2048]); bG = din("bG", [128, 16])
    wbh = din("wbh", [D, D]); wbn = din("wbn", [D, D]); wout = din("wout", [D, D])
    lnd = {nm: din(nm, [128, D]) for nm in ("ln1_g", "ln1_b", "ln2_g", "ln2_b")}
    wr = din("wr", [D, 36]); br = din("br", [128, 36])
    ew1 = din("ew1", [32, D, 512]); ew3 = din("ew3", [32, D, 512]); ew2 = din("ew2", [32, 512, D])
    ident_d = din("ident", [128, 128])
    out_o = nc.dram_tensor("out", [TT, D], F32, kind="ExternalOutput").ap()
    sb, ps = P.sb, P.ps
    hsel = sb("hsel", [128, 2], F32); xa = sb("xa", [128, 512], BF16); xb = sb("xb", [128, 512], BF16)
    big = sb("big", [128, 40960], BF16)
    wGb = big[:, 0:16384].rearrange("p (k c) -> p k c", k=8)
    wbhb = big[:, 16384:24576].rearrange("p (k c) -> p k c", k=8)
    wbnb = big[:, 24576:32768].rearrange("p (k c) -> p k c", k=8)
    woutb = big[:, 32768:40960].rearrange("p (k c) -> p k c", k=8)
    wrf = sb("wrf", [128, 8, 36], F32); brs = sb("brs", [128, 36], F32)
    bGs = sb("bGs", [128, 16], F32)
    identb = sb("identb", [128, 128], BF16); identf = sb("identf", [128, 128], F32); onesf = sb("onesf", [128, 128], F32)
    silc = sb("silc", [128, 8], F32); silcB = sb("silcB", [128, 8, 128], F32)
    modT = sb("modT", [128, 48], F32); scp1 = sb("scp1", [128, 8], F32); scp2 = sb("scp2", [128, 8], F32)
    g1b = sb("g1b", [128, D], F32); g2b = sb("g2b", [128, D], F32)
    lnt = {nm: sb("t_" + nm, [128, D], F32) for nm in lnd}
    stg = sb("stg", [128, 1024], F32)
    xt = sb("xt", [128, D], F32); xr = xt; xn = sb("xn", [128, 4, D], BF16); stat = sb("stat", [128, 16], F32)
    hT = sb("hT", [128, 8, 512], BF16); h2T = hT; h2f = stg
    h2T32 = sb("h2T32", [128, 8, 128], F32)
    oh = sb("oh", [128, 8, 512], BF16); on = sb("on", [128, 8, 512], BF16)
    sgh = sb("sgh", [128, 512], F32); sgn = sb("sgn", [128, 512], F32)
    mT = sb("mT", [128, 8, 512], BF16)
    zt = sb("zt", [128, D], F32); x1 = sb("x1", [128, 4, D], F32)
    lg = sb("lg", [128, 36], F32); rt = sb("rt", [128, 48], F32); m8 = sb("m8", [128, 8], F32)
    elm = sb("elm", [128, 32], F32); eq = sb("eq", [128, 32], F32); C = sb("C", [128, 4, 32], F32)
    ew1b = [big[:, i * 12288:i * 12288 + 4096].rearrange("p (k c) -> p k c", k=8) for i in range(2)]
    ew3b = [big[:, i * 12288 + 4096:i * 12288 + 8192].rearrange("p (k c) -> p k c", k=8) for i in range(2)]
    ew2b = [big[:, i * 12288 + 8192:i * 12288 + 12288].rearrange("p (k c) -> p k c", k=4) for i in range(2)]
    s1 = sgh; aT = mT
    yacc = sb("yacc", [128, 4, D], F32)
    B = [ps("B%d" % i, [128, 512], F32) for i in range(6)]
    TB = [ps("TB%d" % i, [128, 1024], BF16) for i in range(2)]

    w.memset(onesf[:], 1.0, ["onesf"])
    w.ld(identb[:], ident_d, ["identb"], eng="pool"); w.ld(identf[:], ident_d, ["identf"])
    w.ld(hsel[:], hsel_d, ["hsel"]); w.ld(silc[:], cT, ["silc"]); w.ld(modT[:], adabT, ["adabT_s"]); w.ld(bGs[:], bG, ["bGs"]); w.ld(brs[:], br, ["brs"])
    for nm in lnd:
        w.ld(lnt[nm][:], lnd[nm], ["t_" + nm])
    for kc in range(8):
        rs = slice(kc * 128, (kc + 1) * 128)
        w.ld(wrf[:, kc, :], wr[rs, :], ["wrf"])
    w.act(silc[:], silc[:], AF.Silu, ["silc"], ["silc"])
    for kc in range(8):
        w.ts(silcB[:, kc, :], onesf[:], silc[:, kc:kc + 1], None, ALU.mult, ALU.bypass, ["onesf", "silc"], ["silcB"])
    for m in range(48):
        w.ld(stg[:, 0:1024].rearrange("p (k c) -> p k c", k=8), adaw[:, m * 128:(m + 1) * 128].rearrange("(k p) c -> p k c", p=128), ["stg"])
        for kc in range(8):
            w.mm(B[0][:, m:m + 1], stg[:, kc * 128:(kc + 1) * 128], silc[:, kc:kc + 1], kc == 0, kc == 7, ["stg", "silc"], ["B0"])
    w.tt(modT[:], modT[:], B[0][:, 0:48], ALU.add, ["adabT_s", "B0"], ["modT"])
    w.ts(scp1[:], modT[:, 8:16], 1.0, None, ALU.add, ALU.bypass, ["modT"], ["scp1"])
    w.ts(scp2[:], modT[:, 32:40], 1.0, None, ALU.add, ALU.bypass, ["modT"], ["scp2"])
    for (dst, c0, nm) in ((g1b, 2048, "g1b"), (g2b, 5120, "g2b")):
        w.ld(dst[:, :], adabB[:, c0:c0 + 1024], [nm])
        for pc in range(8):
            cs = slice(c0 + pc * 128, c0 + pc * 128 + 128)
            w.ld(stg[:, 0:1024].rearrange("p (k c) -> p k c", k=8), adaw[:, cs].rearrange("(k p) c -> p k c", p=128), ["stg"])
            for kc in range(8):
                w.mm(B[1][:, 0:128], silcB[:, kc, :], stg[:, kc * 128:(kc + 1) * 128], kc == 0, kc == 7, ["silcB", "stg"], ["B1"])
            w.stt(dst[:, pc * 128:(pc + 1) * 128], dst[:, pc * 128:(pc + 1) * 128], 1.0, B[1][:, 0:128], ALU.add, ALU.add, [nm, "B1"], [nm])

    def layer_norm_stats(src_ap, rd):
        P.op("dve", lambda e: e.bn_stats(out=stat[:, 0:6], in_=src_ap[:, 0:512]), rd, ["stat"])
        P.op("dve", lambda e: e.bn_stats(out=stat[:, 6:12], in_=src_ap[:, 512:1024]), rd, ["stat"])
        P.op("dve", lambda e: e.bn_aggr(out=stat[:, 12:14], in_=stat[:, 0:12].rearrange("p (a b) -> p a b", a=2)), ["stat"], ["stat"])
        w.act(stat[:, 14:15], stat[:, 13:14], AF.Ln, ["stat"], ["stat"], bias=1e-5)
        w.act(stat[:, 14:15], stat[:, 14:15], AF.Exp, ["stat"], ["stat"], scale=-0.5)
        w.stt(stat[:, 15:16], stat[:, 12:13], -1.0, stat[:, 14:15], ALU.mult, ALU.mult, ["stat"], ["stat"])

    def load_expert(e, bi):
        for kc in range(8):
            rs = slice(kc * 128, (kc + 1) * 128)
            w.ld(ew1b[bi][:, kc, :], ew1[e, rs, :], ["E%d_1_%d" % (bi, kc)], eng="pool", reads=["P1DONE"])
            w.ld(ew3b[bi][:, kc, :], ew3[e, rs, :], ["E%d_3_%d" % (bi, kc)], eng="pool", reads=["P1DONE"])
        for kc in range(4):
            w.ld(ew2b[bi][:, kc, :], ew2[e, kc * 128:(kc + 1) * 128, :], ["E%d_2_%d" % (bi, kc)], eng="pool", reads=["P1DONE"])

    def load_weights():
        for kc in range(8):
            rs = slice(kc * 128, (kc + 1) * 128)
            w.ld(wGb[:, kc, :], wG[rs, :], ["WG%d" % kc], eng="pool", reads=["MOEDONE"]); w.ld(wbhb[:, kc, :], wbh[rs, :], ["WH%d" % kc], eng="pool", reads=["MOEDONE"])
            w.ld(wbnb[:, kc, :], wbn[rs, :], ["WN%d" % kc], eng="pool", reads=["MOEDONE"]); w.ld(woutb[:, kc, :], wout[rs, :], ["WO%d" % kc], eng="pool", reads=["MOEDONE"])

    for J in range(NB):
        t0 = J * 512
        load_weights()
        for (dst, src, nm) in ((oh, ohT, "oh"), (on, onT, "on")):
            for k in range(8):
                w.ld(xa[:], src[k * 128:(k + 1) * 128, t0:t0 + 512], ["xa"])
                w.ld(xb[:], src[k * 128:(k + 1) * 128, TT + t0:TT + t0 + 512], ["xb"])
                w.ts(dst[:, k, :], xa[:], hsel[:, 0:1], None, ALU.mult, ALU.bypass, ["xa", "hsel"], [nm])
                w.stt(dst[:, k, :], xb[:], hsel[:, 1:2], dst[:, k, :], ALU.mult, ALU.add, ["xb", "hsel", nm], [nm])
        for s4 in range(4):
            w.ld(xt[:], x[t0 + s4 * 128:t0 + (s4 + 1) * 128, :], ["xt"])
            layer_norm_stats(xt[:, :], ["xt"])
            w.act(xn[:, s4, :], xt[:], AF.Identity, ["xt", "stat"], ["xn"], bias=stat[:, 15:16], scale=stat[:, 14:15])
        for c in range(8):
            tb = TB[c % 2]
            for s4 in range(4):
                w.tr(tb[:, s4 * 128:(s4 + 1) * 128], xn[:, s4, c * 128:(c + 1) * 128], identb[:], ["xn", "identb"], ["TB%d" % (c % 2)])
            w.ts(hT[:, c, :], tb[:, 0:512], scp1[:, c:c + 1], modT[:, c:c + 1], ALU.mult, ALU.add, ["TB%d" % (c % 2), "scp1", "modT"], ["hT"])
        for c in range(8):
            cs = slice(c * 128, (c + 1) * 128)
            for kc in range(8):
                w.mm(B[0][:, :], wGb[:, kc, cs], hT[:, kc, :], kc == 0, kc == 7, ["WG%d" % kc, "hT"], ["B0"])
            w.act(sgh[:], B[0][:, :], AF.Sigmoid, ["B0", "bGs"], ["sgh"], bias=bGs[:, c:c + 1])
            for kc in range(8):
                w.mm(B[1][:, :], wGb[:, kc, 1024 + c * 128:1024 + (c + 1) * 128], hT[:, kc, :], kc == 0, kc == 7, ["WG%d" % kc, "hT"], ["B1"])
            w.act(sgn[:], B[1][:, :], AF.Sigmoid, ["B1", "bGs"], ["sgn"], bias=bGs[:, 8 + c:9 + c])
            for kc in range(8):
                w.mm(B[2][:, :], wbhb[:, kc, cs], oh[:, kc, :], kc == 0, kc == 7, ["WH%d" % kc, "oh"], ["B2"])
            for kc in range(8):
                w.mm(B[3][:, :], wbnb[:, kc, cs], on[:, kc, :], kc == 0, kc == 7, ["WN%d" % kc, "on"], ["B3"])
            w.tt(sgh[:], sgh[:], B[2][:, :], ALU.mult, ["sgh", "B2"], ["sgh"])
            w.tt(sgn[:], sgn[:], B[3][:, :], ALU.mult, ["sgn", "B3"], ["sgn"])
            w.tt(mT[:, c, :], sgh[:], sgn[:], ALU.add, ["sgh", "sgn"], ["mT"], eng="pool")
        for s4 in range(4):
            tsl = slice(s4 * 128, (s4 + 1) * 128)
            w.ld(xr[:], x[t0 + s4 * 128:t0 + (s4 + 1) * 128, :], ["xt"])
            for hf in range(2):
                fs = slice(hf * 512, (hf + 1) * 512)
                for kc in range(8):
                    w.mm(B[4][:, :], mT[:, kc, tsl], woutb[:, kc, fs], kc == 0, kc == 7, ["mT", "WO%d" % kc], ["B4"])
                w.tt(zt[:, fs], B[4][:, :], g1b[:, fs], ALU.mult, ["B4", "g1b"], ["zt"])
                w.stt(zt[:, fs], xr[:, fs], ALPHA_C, zt[:, fs], ALU.mult, ALU.add, ["xt", "zt"], ["zt"])
            layer_norm_stats(zt[:, :], ["zt"])
            w.act(zt[:], zt[:], AF.Identity, ["zt", "stat"], ["zt"], bias=stat[:, 15:16], scale=stat[:, 14:15])
            w.tt(zt[:], zt[:], lnt["ln1_g"][:], ALU.mult, ["zt", "t_ln1_g"], ["zt"])
            w.tt(x1[:, s4, :], zt[:], lnt["ln1_b"][:], ALU.add, ["zt", "t_ln1_b"], ["x1", "P1DONE"] if s4 == 3 else ["x1"])
            layer_norm_stats(x1[:, s4, :], ["x1"])
            w.act(h2f[:], x1[:, s4, :], AF.Identity, ["x1", "stat"], ["stg"], bias=stat[:, 15:16], scale=stat[:, 14:15])
            w.cp(xn[:, s4, :], h2f[:], ["stg"], ["xn"], eng="pool")
            for c in range(8):
                w.tr(B[5][:, 0:128], h2f[:, c * 128:(c + 1) * 128], identf[:], ["stg", "identf"], ["B5"])
                w.ts(h2T32[:, c, :], B[5][:, 0:128], scp2[:, c:c + 1], modT[:, 24 + c:25 + c], ALU.mult, ALU.add, ["B5", "scp2", "modT"], ["h2T32"])
            for kc in range(8):
                w.mm(B[5][:, 128:164], h2T32[:, kc, :], wrf[:, kc, :], kc == 0, kc == 7, ["h2T32", "wrf"], ["B5"])
            w.tt(lg[:], B[5][:, 128:164], brs[:], ALU.add, ["B5", "brs"], ["lg"])
            P.op("dve", lambda e: e.tensor_reduce(out=rt[:, 0:1], in_=lg[:, 0:4], axis=AX.X, op=ALU.max), ["lg"], ["rt"])
            w.ts(rt[:, 1:2], rt[:, 0:1], -1.0, None, ALU.mult, ALU.bypass, ["rt"], ["rt"])
            w.act(rt[:, 4:8], lg[:, 0:4], AF.Exp, ["lg", "rt"], ["rt"], bias=rt[:, 1:2])
            P.op("dve", lambda e: e.tensor_reduce(out=rt[:, 2:3], in_=rt[:, 4:8], axis=AX.X, op=ALU.add), ["rt"], ["rt"])
            P.op("dve", lambda e: e.reciprocal(out=rt[:, 3:4], in_=rt[:, 2:3]), ["rt"], ["rt"])
            w.ts(rt[:, 8:12], lg[:, 0:4], rt[:, 0:1], None, ALU.is_ge, ALU.bypass, ["lg", "rt"], ["rt"])
            w.ts(rt[:, 8:12], rt[:, 8:12], 1e9, -1e9, ALU.mult, ALU.add, ["rt"], ["rt"])
            for g in range(4):
                w.ts(elm[:, 8 * g:8 * g + 8], lg[:, 4 + 8 * g:12 + 8 * g], rt[:, 8 + g:9 + g], None, ALU.add, ALU.bypass, ["lg", "rt"], ["elm"])
            P.op("dve", lambda e: e.max(out=m8[:, 0:8], in_=elm[:]), ["elm"], ["m8"])
            w.tt(rt[:, 12:13], m8[:, 1:2], m8[:, 0:1], ALU.subtract, ["m8"], ["rt"])
            w.act(rt[:, 12:13], rt[:, 12:13], AF.Exp, ["rt"], ["rt"])
            w.ts(rt[:, 12:13], rt[:, 12:13], 1.0, None, ALU.add, ALU.bypass, ["rt"], ["rt"])
            P.op("dve", lambda e: e.reciprocal(out=rt[:, 13:14], in_=rt[:, 12:13]), ["rt"], ["rt"])
            w.ts(rt[:, 14:15], rt[:, 13:14], -1.0, 1.0, ALU.mult, ALU.add, ["rt"], ["rt"])
            w.tt(rt[:, 13:14], rt[:, 13:14], rt[:, 3:4], ALU.mult, ["rt"], ["rt"])
            w.tt(rt[:, 14:15], rt[:, 14:15], rt[:, 3:4], ALU.mult, ["rt"], ["rt"])
            w.ts(eq[:], elm[:], m8[:, 0:1], rt[:, 13:14], ALU.is_equal, ALU.mult, ["elm", "m8", "rt"], ["eq"])
            w.ts(C[:, s4, :], elm[:], m8[:, 1:2], rt[:, 14:15], ALU.is_equal, ALU.mult, ["elm", "m8", "rt"], ["C"])
            w.tt(C[:, s4, :], C[:, s4, :], eq[:], ALU.add, ["C", "eq"], ["C"])
        for c in range(8):
            tb = TB[c % 2]
            for s4 in range(4):
                w.tr(tb[:, s4 * 128:(s4 + 1) * 128], xn[:, s4, c * 128:(c + 1) * 128], identb[:], ["xn", "identb"], ["TB%d" % (c % 2)])
            w.ts(h2T[:, c, :], tb[:, 0:512], scp2[:, c:c + 1], modT[:, 24 + c:25 + c], ALU.mult, ALU.add, ["TB%d" % (c % 2), "scp2", "modT"], ["hT"])
        load_expert(0, 0)
        for e in range(n_exp):
            bi = e % 2
            if e + 1 < n_exp:
                load_expert(e + 1, 1 - bi)
            for hc in range(4):
                hs = slice(hc * 128, (hc + 1) * 128)
                ba, bb = (0, 1) if hc % 2 == 0 else (4, 5)
                for kc in range(8):
                    w.mm(B[ba][:, :], ew1b[bi][:, kc, hs], h2T[:, kc, :], kc == 0, kc == 7, ["E%d_1_%d" % (bi, kc), "hT"], ["B%d" % ba])
                for kc in range(8):
                    w.mm(B[bb][:, :], ew3b[bi][:, kc, hs], h2T[:, kc, :], kc == 0, kc == 7, ["E%d_3_%d" % (bi, kc), "hT"], ["B%d" % bb])
                sbuf1 = s1 if hc % 2 == 0 else sgn
                skey = "sgh" if hc % 2 == 0 else "sgn"
                w.act(sbuf1[:], B[ba][:, :], AF.Silu, ["B%d" % ba], [skey])
                w.tt(aT[:, hc, :], sbuf1[:], B[bb][:, :], ALU.mult, [skey, "B%d" % bb], ["mT"])
            for s4 in range(4):
                tsl = slice(s4 * 128, (s4 + 1) * 128)
                for hf in range(2):
                    fs = slice(hf * 512, (hf + 1) * 512)
                    bk = 2 + (s4 * 2 + hf) % 2
                    for kc in range(4):
                        w.mm(B[bk][:, :], aT[:, kc, tsl], ew2b[bi][:, kc, fs], kc == 0, kc == 3, ["mT", "E%d_2_%d" % (bi, kc)], ["B%d" % bk])
                    if e == 0:
                        w.ts(yacc[:, s4, fs], B[bk][:, :], C[:, s4, e:e + 1], None, ALU.mult, ALU.bypass, ["B%d" % bk, "C"], ["yacc"])
                    else:
                        w.stt(yacc[:, s4, fs], B[bk][:, :], C[:, s4, e:e + 1], yacc[:, s4, fs], ALU.mult, ALU.add, ["B%d" % bk, "C", "yacc"],
                              ["yacc", "MOEDONE"] if (e == n_exp - 1 and s4 == 3 and hf == 1) else ["yacc"])
        for s4 in range(4):
            w.tt(zt[:], yacc[:, s4, :], g2b[:], ALU.mult, ["yacc", "g2b"], ["zt"])
            w.stt(zt[:], x1[:, s4, :], ALPHA_C, zt[:], ALU.mult, ALU.add, ["x1", "zt"], ["zt"])
            layer_norm_stats(zt[:, :], ["zt"])
            w.act(zt[:], zt[:], AF.Identity, ["zt", "stat"], ["zt"], bias=stat[:, 15:16], scale=stat[:, 14:15])
            w.tt(zt[:], zt[:], lnt["ln2_g"][:], ALU.mult, ["zt", "t_ln2_g"], ["zt"])
            w.tt(zt[:], zt[:], lnt["ln2_b"][:], ALU.add, ["zt", "t_ln2_b"], ["zt"])
            w.st(out_o[t0 + s4 * 128:t0 + (s4 + 1) * 128, :], zt[:], ["zt"], is_out=True)


def build_fused(T, n_exp=32):
    nc = bass.Bass("TRN2", target_bir_lowering=False)
    P = Prog(nc)
    w = W(P)
    ohT_x = nc.dram_tensor("xch_oh", [D, T], BF16, kind="Internal").ap()
    onT_x = nc.dram_tensor("xch_on", [D, T], BF16, kind="Internal").ap()
    P.pfx = "a_"
    emit_p1(nc, P, w, T, ohT_x, onT_x)
    P.barrier()
    P.release_tensors()
    P.pfx = "b_"
    emit_p2(nc, P, w, T // 2, ohT_x, onT_x, n_exp)
    P.finish()
    return nc


def host_inputs_fused(inp, b, s, T):
    d = {}
    for g in range(2):
        dg = host_inputs_p1(inp, b, g, T)
        for k, v in dg.items():
            if k in PER_GROUP:
                d["g%d_%s" % (g, k)] = v
            elif g == 0:
                d[k] = v
    d2 = host_inputs_p2(inp, b, s, T // 2)
    for k, v in d2.items():
        d["q_" + k] = v
    hs = np.zeros((128, 2), np.float32)
    hs[:, s] = 1.0
    d["q_hsel"] = hs
    return d


def kernel(**inputs):
    inp = {k: np.asarray(v) for k, v in inputs.items()}
    T = inp["x"].shape[1]
    nb = inp["x"].shape[0]
    ncores = 2 * nb
    nc = build_fused(T)
    maps = [host_inputs_fused(inp, c // 2, c % 2, T) for c in range(ncores)]
    r = run_bass_kernel_spmd(nc, maps, core_ids=list(range(ncores)))
    out = np.stack([np.concatenate([np.asarray(r.results[2 * b]["out"]), np.asarray(r.results[2 * b + 1]["out"])], axis=0) for b in range(nb)])
    return out.astype(np.float32)
```

```python
import numpy as np
import concourse.bass as bass
import concourse.mybir as mybir
from contextlib import ExitStack

F32 = mybir.dt.float32
BF16 = mybir.dt.bfloat16
I32 = mybir.dt.int32
U32 = mybir.dt.uint32
AF = mybir.ActivationFunctionType
ALU = mybir.AluOpType
AX = mybir.AxisListType

ENGS = ("pe", "act", "dve", "pool", "sp")
NDMA = 6


class Prog:
    def __init__(self, nc, same_engine_sync=True):
        self.nc = nc
        self.es = ExitStack()
        self.ops = {e: [] for e in ENGS}
        self.cnt = {e: 0 for e in ENGS}
        self.sem = {e: self.es.enter_context(nc.semaphore("s_" + e)) for e in ENGS}
        self.dsem = {}
        self.dcnt = {}
        self.dlast = {}
        self.drr = {}
        for e in ("sp", "pool", "act"):
            self.dsem[e] = [self.es.enter_context(nc.semaphore("d_%s%d" % (e, i))) for i in range(NDMA)]
            self.dcnt[e] = [0] * NDMA
            self.drr[e] = 0
        self.seen = {e: {} for e in ENGS}
        self.lastw = {}
        self.readers = {}
        self.same = same_engine_sync
        self.out_tokens = []
        self.n_sb = 0
        self.pfx = ""
        self.ts_ = ExitStack()

    def sb(self, name, shape, dt):
        return self.ts_.enter_context(self.nc.sbuf_tensor("sb_" + self.pfx + name, list(shape), dt))

    def ps(self, name, shape, dt):
        return self.ts_.enter_context(self.nc.psum_tensor("ps_" + self.pfx + name, list(shape), dt))

    def release_tensors(self):
        self.ts_.close()
        self.ts_ = ExitStack()

    def barrier(self):
        toks = []
        for e in ENGS:
            if self.cnt[e] > 0:
                toks.append(("s_" + e, self.sem[e], self.cnt[e]))
        for e in self.dsem:
            for j in range(NDMA):
                if self.dcnt[e][j] > 0:
                    toks.append(("d_%s%d" % (e, j), self.dsem[e][j], self.dcnt[e][j]))
        for e in ENGS:
            waits = []
            for (sname, sh, val) in toks:
                if sname == "s_" + e:
                    continue
                if self.seen[e].get(sname, 0) >= val:
                    continue
                waits.append((sh, val))
                self.seen[e][sname] = val
            self.ops[e].append((waits, None, None, 0))

    def _deps(self, eng, reads, writes, is_mm=False):
        toks = []
        for k in reads:
            w = self.lastw.get(k)
            if w is not None:
                toks.append(w)
        for k in writes:
            w = self.lastw.get(k)
            if w is not None:
                toks.append(w)
            for r in self.readers.get(k, {}).values():
                toks.append(r)
        need = {}
        for (sname, sh, val, teng) in toks:
            if teng == eng and sname.startswith("s_"):
                if not self.same or (is_mm and eng == "pe"):
                    continue
            if self.seen[eng].get(sname, 0) >= val:
                continue
            if need.get(sname, (None, 0))[1] < val:
                need[sname] = (sh, val)
        for sname, (sh, val) in need.items():
            self.seen[eng][sname] = val
        return list(need.values())

    def _record(self, tok, reads, writes):
        for k in writes:
            self.lastw[k] = tok
            self.readers[k] = {}
        for k in reads:
            d = self.readers.setdefault(k, {})
            key = tok[0]
            if key not in d or d[key][2] < tok[2]:
                d[key] = tok

    def op(self, eng, fn, reads=(), writes=(), is_mm=False):
        waits = self._deps(eng, reads, writes, is_mm)
        self.cnt[eng] += 1
        tok = ("s_" + eng, self.sem[eng], self.cnt[eng], eng)
        self.ops[eng].append((waits, fn, self.sem[eng], 1))
        self._record(tok, reads, writes)
        return tok

    def dma(self, eng, fn, reads=(), writes=(), is_out=False):
        j = self.drr[eng]
        self.drr[eng] = (j + 1) % NDMA
        sh = self.dsem[eng][j]
        sname = "d_%s%d" % (eng, j)
        waits = self._deps(eng, reads, writes)
        prev = self.dcnt[eng][j]
        if prev > 0 and self.seen[eng].get(sname, 0) < prev:
            waits.append((sh, prev))
            self.seen[eng][sname] = prev
        self.dcnt[eng][j] += 16
        tok = (sname, sh, self.dcnt[eng][j], "dma_" + eng)
        self.ops[eng].append((waits, fn, sh, 16))
        self._record(tok, reads, writes)
        if is_out:
            self.out_tokens.append(tok)
        return tok

    def finish(self):
        nc = self.nc
        final_waits = {}
        for (sname, sh, val, _) in self.out_tokens:
            if final_waits.get(sname, (None, 0))[1] < val:
                final_waits[sname] = (sh, val)
        fw = list(final_waits.values())
        engmap = {"pe": "tensor", "act": "scalar", "dve": "vector", "pool": "gpsimd", "sp": "sync"}
        with nc.Block() as block:
            for e in ENGS:
                ops = self.ops[e]
                extra = fw if e == "sp" else []

                def body(engine, ops=ops, extra=extra):
                    for (waits, fn, sh, inc) in ops:
                        for (wsh, wval) in waits:
                            engine.wait_ge(wsh, wval)
                        if fn is None:
                            continue
                        ins = fn(engine)
                        ins.then_inc(sh, inc)
                    for (wsh, wval) in extra:
                        engine.wait_ge(wsh, wval)

                getattr(block, engmap[e])(body)
        self.ts_.close()
        self.es.close()


import os
from concourse.bass_utils import run_bass_kernel_spmd

D = 1024
NEG = -30000.0
SCALE = 128 ** -0.5


class W:
    def __init__(self, P):
        self.P = P

    def mm(self, out, lhsT, rhs, start, stop, reads, writes):
        return self.P.op("pe", lambda e: e.matmul(out, lhsT, rhs, start=start, stop=stop), reads, writes, is_mm=True)

    def tr(self, out, in_, ident, reads, writes):
        return self.P.op("pe", lambda e: e.transpose(out, in_, ident), reads, writes, is_mm=True)

    def act(self, out, in_, func, reads, writes, bias=0.0, scale=1.0):
        return self.P.op("act", lambda e: e.activation(out=out, in_=in_, func=func, bias=bias, scale=scale), reads, writes)

    def ts(self, out, in0, s1, s2, op0, op1, reads, writes, eng="dve"):
        return self.P.op(eng, lambda e: e.tensor_scalar(out=out, in0=in0, scalar1=s1, scalar2=s2, op0=op0, op1=op1), reads, writes)

    def tt(self, out, in0, in1, op, reads, writes, eng="dve"):
        return self.P.op(eng, lambda e: e.tensor_tensor(out=out, in0=in0, in1=in1, op=op), reads, writes)

    def stt(self, out, in0, scalar, in1, op0, op1, reads, writes):
        return self.P.op("dve", lambda e: e.scalar_tensor_tensor(out=out, in0=in0, scalar=scalar, in1=in1, op0=op0, op1=op1), reads, writes)

    def cp(self, out, in_, reads, writes, eng="dve"):
        if eng == "act":
            return self.P.op("act", lambda e: e.copy(out=out, in_=in_), reads, writes)
        return self.P.op(eng, lambda e: e.tensor_copy(out=out, in_=in_), reads, writes)

    def memset(self, ap, val, writes, eng="pool"):
        return self.P.op(eng, lambda e: e.memset(ap, val), (), writes)

    def ld(self, out, in_, writes, eng="sp", reads=()):
        return self.P.dma(eng, lambda e: e.dma_start(out=out, in_=in_), reads, writes)

    def st(self, out, in_, reads, writes=(), is_out=False, eng="sp"):
        return self.P.dma(eng, lambda e: e.dma_start(out=out, in_=in_), reads, writes, is_out=is_out)


def rel_bucket_np(dist):
    n = np.maximum(dist, 0)
    nf = np.maximum(n, 1).astype(np.float32)
    large = 16 + (np.log(nf / np.float32(16)) / np.float32(np.log(128 / 16)) * np.float32(16)).astype(np.int32)
    large = np.minimum(large, 31)
    return np.where(n < 16, n, large)


def host_consts(T):
    c = {}
    c["ident"] = np.eye(128, dtype=np.float32)
    j = np.arange(128)
    c["triU"] = (j[:, None] <= j[None, :]).astype(np.float32)
    kk = np.arange(2048)
    c["H32"] = (np.arange(32)[:, None] == (kk[None, :] // 64) % 32).astype(np.float32)
    NCT = max(1, T // 2048)
    n = np.arange(NCT * 128)
    jj = np.arange(128)
    ov = ((16 * n[:, None] < 64 * jj[None, :] + 64) & (16 * n[:, None] + 32 > 64 * jj[None, :])).astype(np.float32)
    ov[n >= T // 16 - 1] = 0.0
    c["ovm"] = ov.reshape(NCT, 128, 128).transpose(1, 0, 2).copy()
    qi = np.arange(128)
    cq = (qi >= 64).astype(np.int64)
    rel = np.arange(256) - 128
    A = (rel[None, :] < cq[:, None] - 1).astype(np.float32)
    Bm = np.where(rel[None, :] > cq[:, None], -1.0,
                  np.where(rel[None, :] >= cq[:, None] - 1, 1e4, 0.0)).astype(np.float32)
    c["selA"] = A
    c["selB"] = Bm
    sr = np.zeros((12, 12, 128), np.float32)
    for r in range(12):
        sr[r, r, :] = 1.0
    c["selrow"] = sr.reshape(12, 12 * 128)
    ki = np.arange(128)[:, None]
    qq = np.arange(128)[None, :]
    md = np.zeros((4, 128, 128), np.float32)
    md[0] = np.where(qq - ki >= 0, 0.0, NEG)
    md[2] = np.where(512 + qq - ki < 512, 0.0, NEG)
    md[3] = NEG
    c["maskD"] = md
    ni = np.arange(128)[:, None]
    q5 = np.arange(512)[None, :]
    mc = np.zeros((5, 128, 512), np.float32)
    for o in range(5):
        mc[o] = np.where(512 * o + q5 - 16 * ni - 31 >= 0, 0.0, NEG)
    c["maskC"] = mc
    return c


def host_bias_tables(rel_bias_g):
    ki = np.arange(128)[:, None]
    qq = np.arange(128)[None, :]
    bd = np.zeros((4, 4, 128, 128), np.float32)
    for k, off in enumerate((0, 128, 512)):
        bk = rel_bucket_np(off + qq - ki)
        bd[:, k] = rel_bias_g[:, bk]
    bd[:, 3] = rel_bias_g[:, 31][:, None, None]
    ni = np.arange(128)[:, None]
    q5 = np.arange(512)[None, :]
    bc = np.zeros((4, 5, 128, 512), np.float32)
    for o in range(5):
        bk = rel_bucket_np(512 * o + q5 - 16 * ni - 31)
        bc[:, o] = rel_bias_g[:, bk]
    c31 = np.broadcast_to(rel_bias_g[:, 31][None, :], (128, 4)).copy()
    return bd, bc, c31


def host_inputs_p1(inp, b, g, T):
    S = np.cumsum([0, 1024, 1024, 1024, 1024, 1024, 256, 256, 256, 256, 256, 256, 24, 1024, 1024])
    w_in = inp["w_in"][0]
    b_in = inp["b_in"][0]
    hs = slice(512 * g, 512 * g + 512)
    gs = slice(128 * g, 128 * g + 128)
    colsF = np.concatenate([np.arange(S[0], S[1])[hs], np.arange(S[1], S[2])[hs], np.arange(S[3], S[4])[hs],
                            np.arange(S[4], S[5])[hs], np.arange(S[5], S[6])[gs], np.arange(S[6], S[7])[gs],
                            np.arange(S[7], S[8])[gs], np.arange(S[9], S[10])[gs],
                            np.arange(S[11], S[12])[12 * g:12 * g + 12]])
    colsT = np.concatenate([np.arange(S[2], S[3])[hs], np.arange(S[8], S[9])[gs], np.arange(S[10], S[11])[gs]])
    wF = np.zeros((1024, 2688), np.float32)
    wF[:, :2572] = w_in[:, colsF]
    bFv = np.zeros((2688,), np.float32)
    bFv[:2572] = b_in[colsF]
    d = {}
    d["x"] = np.ascontiguousarray(inp["x"][b, :T])
    d["cT"] = np.ascontiguousarray(inp["c"][b].reshape(8, 128).T)
    d["adaw"] = np.ascontiguousarray(inp["ada_w"][0][:, 0:2048])
    d["adab"] = np.ascontiguousarray(inp["ada_b"][0][0:2048].reshape(16, 128).T)
    d["wF"] = wF
    d["bF"] = np.ascontiguousarray(bFv.reshape(21, 128).T)
    d["wT"] = np.ascontiguousarray(w_in[:, colsT])
    d["bT"] = np.ascontiguousarray(b_in[colsT][None, :])
    lg = inp["hg_lb_logits"]
    d["lbl"] = np.ascontiguousarray(np.concatenate([lg[0, hs].reshape(4, 128).T, lg[1, hs].reshape(4, 128).T], axis=1))
    d["normw"] = np.ascontiguousarray(inp["hg_norm_w"][0][hs].reshape(4, 128).T)
    for s in ("k", "v"):
        d["pos" + s] = np.ascontiguousarray(inp["cmp_pos_" + s][0].T)
        d["w1" + s] = np.ascontiguousarray(inp["cmp_w1_" + s][0])
        d["b1" + s] = np.ascontiguousarray(inp["cmp_b1_" + s][0][:, None])
        d["w2" + s] = np.ascontiguousarray(inp["cmp_w2_" + s][0])
    bd, bc, c31 = host_bias_tables(inp["rel_bias"][4 * g:4 * g + 4])
    d["biasD"], d["biasC"], d["c31"] = bd, bc, c31
    d.update(host_consts(T))
    return d


PER_GROUP = ("wF", "bF", "wT", "bT", "lbl", "normw", "biasD", "biasC", "c31")


def emit_p1(nc, P, w, T, ohT_o, onT_o, stage=99):
    NB = T // 512
    NT = T // 128
    NCT = max(1, T // 2048)
    sb, ps = P.sb, P.ps
    _cache = {}

    def dbg(*a, **k):
        return None

    wFb = sb("wFb", [128, 8, 2688], BF16); wTb = sb("wTb", [128, 8, 768], BF16)
    bFs = sb("bFs", [128, 21], F32); bTb = sb("bTb", [1, 768], BF16)
    identb = sb("identb", [128, 128], BF16); triU = sb("triU", [128, 128], F32)
    onesb = sb("onesb", [128, 128], BF16); onesf = sb("onesf", [128, 128], F32)
    H32 = sb("H32", [32, 2048], BF16); ovm = sb("ovm", [128, NCT, 128], BF16)
    selA = sb("selA", [128, 256], F32); selB = sb("selB", [128, 256], F32); selrow = sb("selrow", [12, 12 * 128], F32)
    Dt = sb("Dt", [128, 4, 4, 128], BF16)
    D4t = sb("D4t", [128, 4, 128], BF16)
    c31 = sb("c31", [128, 4], F32); nc31 = sb("nc31", [128, 4], F32)
    stg = sb("stg", [128, 1024], F32)
    cbt = [sb("cbt%d" % i, [128, 512], BF16) for i in range(2)]
    mod1 = sb("mod1", [128, 16], F32); scp1 = sb("scp1", [128, 8], F32)
    silc = sb("silc", [128, 8], F32); bFq = sb("bFq", [128, 4], F32)
    oacc4 = sb("oacc4", [128, 4, 512], F32)
    lb = sb("lb", [128, 4], F32); oml = sb("oml", [128, 4], F32); lbt = sb("lbt", [128, 8], F32); nw = sb("nw", [128, 4], F32)
    cw1 = sb("cw1", [128, 32, 128], BF16)
    cw2 = {s: sb("cw2" + s, [128, 128], BF16) for s in "kv"}
    cpos = {s: sb("cpos" + s, [128, 1], F32) for s in "kv"}
    posb = sb("posb", [128, 32], BF16); b1s = sb("b1s", [128, 2], F32)
    xt = sb("xt", [128, D], F32); xn = sb("xn", [128, 4, D], BF16)
    stat = sb("stat", [128, 16], F32)
    hT = sb("hT", [128, 8, 512], BF16)
    qT = sb("qT", [128, 512], F32); sg = sb("sg", [128, 512], F32); gT = sb("gT", [128, 512], F32)
    t1 = sb("t1", [128, 512], F32); t2 = sb("t2", [128, 512], F32); t3 = sb("t3", [128, 512], F32); t4 = sb("t4", [128, 512], F32)
    qe = sb("qe", [128, 512], BF16); kin = sb("kin", [128, 512], BF16); kendT = sb("kendT", [128, 512], BF16)
    dec = sb("dec", [128, 4], F32)
    kend4 = sb("kend4", [128, 512], BF16); attm4 = sb("attm4", [128, 512], BF16)
    S32 = sb("S32", [128, 4, 128], F32); S16v = sb("S16v", [128, 4, 5, 128], BF16)
    vtk = sb("vtk", [128, 4, 512], BF16)
    oT = stg[:, 0:512]; osq = stg[:, 512:1024]; stg2 = t4; ohb = sb("ohb", [128, 512], BF16)
    nqT = sb("nqT", [128, 4, 512], BF16)
    cext = {s: sb("cext" + s, [128, 528], BF16) for s in "kv"}
    hidT = {s: sb("hidT" + s, [128, NCT * 128], BF16) for s in "kv"}
    kcmpT = sb("kcmpT", [128, NCT * 128], BF16); vcmp = sb("vcmp", [128, NCT, 128], BF16)
    ksT = sb("ksT", [128, T], BF16); vs = sb("vs", [128, NT, 128], BF16)
    kwT = sb("kwT", [128, 8, 128], BF16); vw = sb("vw", [128, 8, 128], BF16)
    gate = sb("gate", [12, 512], F32)
    PT = [sb("PT%d" % i, [128, 512], BF16) for i in range(3)]
    rZ = t1; fac = t2; impacc = t3
    onb = ohb
    rzt = sb("rzt", [128, 16], F32)
    sc = sb("sc", [128, 128], F32); scw = sb("scw", [128, 128], F32)
    m8 = sb("m8", [128, 16], F32); nsel = sb("nsel", [128, 128], BF16)
    nselT = sb("nselT", [32, 4, 512], BF16)
    B = [ps("B%d" % i, [128, 512], F32) for i in range(6)]
    TB = [ps("TB%d" % i, [128, 1024], BF16) for i in range(2)]

    for g in range(2):
        def din(name, shape, dt=F32):
            full = ("g%d_" % g + name) if name in PER_GROUP else name
            if full not in _cache:
                _cache[full] = nc.dram_tensor(full, list(shape), dt, kind="ExternalInput").ap()
            return _cache[full]

        x = din("x", [T, D]); cT = din("cT", [128, 8]); adaw = din("adaw", [D, 2048]); adab = din("adab", [128, 16])
        wF = din("wF", [D, 2688]); bF = din("bF", [128, 21]); wT = din("wT", [D, 768]); bT = din("bT", [1, 768])
        lbl = din("lbl", [128, 8]); normw = din("normw", [128, 4])
        cmpw = {}
        for s in ("k", "v"):
            cmpw[s] = (din("pos" + s, [128, 32]), din("w1" + s, [4096, 128]), din("b1" + s, [128, 1]), din("w2" + s, [128, 128]))
        biasD = din("biasD", [4, 4, 128, 128]); biasC = din("biasC", [4, 5, 128, 512]); c31d = din("c31", [128, 4])
        ident_d = din("ident", [128, 128]); triU_d = din("triU", [128, 128]); H32_d = din("H32", [32, 2048])
        ovm_d = din("ovm", [128, NCT, 128]); selA_d = din("selA", [128, 256]); selB_d = din("selB", [128, 256])
        selrow_d = din("selrow", [12, 12 * 128]); maskD_d = din("maskD", [4, 128, 128]); maskC_d = din("maskC", [5, 128, 512])
        w.memset(onesb[:], 1.0, ["onesb"]); w.memset(onesf[:], 1.0, ["onesf"])
        for (dst, src, nm) in ((identb, ident_d, "identb"), (H32, H32_d, "H32"), (ovm, ovm_d, "ovm"), (bTb, bT, "bTb")):
            w.ld(dst[:], src, [nm], eng="pool")
        for (dst, src, nm) in ((triU, triU_d, "triU"), (selA, selA_d, "selA"), (selB, selB_d, "selB"), (selrow, selrow_d, "selrow"),
                               (c31, c31d, "c31"), (bFs, bF, "bFs"), (lbt, lbl, "lbt"), (nw, normw, "nw"), (silc, cT, "silc"),
                               (mod1, adab, "adab_s")):
            w.ld(dst[:], src, [nm])
        for kc in range(8):
            w.ld(wFb[:, kc, :], wF[kc * 128:(kc + 1) * 128, :], ["wFb"], eng="pool")
            w.ld(wTb[:, kc, :], wT[kc * 128:(kc + 1) * 128, :], ["wTb"], eng="pool")
        w.ts(nc31[:], c31[:], -1.0, None, ALU.mult, ALU.bypass, ["c31"], ["nc31"])
        w.ts(bFq[:], bFs[:, 12:16], SCALE, None, ALU.mult, ALU.bypass, ["bFs"], ["bFq"])
        for h in range(4):
            for k in range(4):
                w.ld(stg[:, 0:128], biasD[h, k], ["stg"])
                w.ld(stg[:, 128:256], maskD_d[k], ["stg"])
                w.stt(Dt[:, h, k, :], stg[:, 0:128], c31[:, h:h + 1], stg[:, 128:256], ALU.subtract, ALU.add, ["stg", "c31"], ["Dt"])
                if k == 2:
                    w.stt(D4t[:, h, :], stg[:, 0:128], c31[:, h:h + 1], stg[:, 128:256], ALU.subtract, ALU.add, ["stg", "c31"], ["Dt"])
        w.tt(lb[:], lbt[:, 0:4], lbt[:, 4:8], ALU.subtract, ["lbt"], ["lb"])
        w.act(lb[:], lb[:], AF.Sigmoid, ["lb"], ["lb"])
        w.ts(oml[:], lb[:], -1.0, 1.0, ALU.mult, ALU.add, ["lb"], ["oml"])
        w.act(silc[:], silc[:], AF.Silu, ["silc"], ["silc"])
        for m in range(16):
            w.ld(stg[:, 0:1024].rearrange("p (k c) -> p k c", k=8), adaw[:, m * 128:(m + 1) * 128].rearrange("(k p) c -> p k c", p=128), ["stg"])
            for kc in range(8):
                w.mm(B[0][:, m:m + 1], stg[:, kc * 128:(kc + 1) * 128], silc[:, kc:kc + 1], kc == 0, kc == 7, ["stg", "silc"], ["B0"])
        w.tt(mod1[:], mod1[:], B[0][:, 0:16], ALU.add, ["adab_s", "B0"], ["mod1"])
        w.ts(scp1[:], mod1[:, 8:16], 1.0, None, ALU.add, ALU.bypass, ["mod1"], ["scp1"])
        for si, s in enumerate("kv"):
            posd, w1d, b1d, w2d = cmpw[s]
            w.ld(posb[:], posd, ["posb"], eng="pool")
            w.ld(cw1[:], w1d.rearrange("(r d) m -> d r m", d=128), ["cw1"], eng="pool")
            w.ld(cw2[s][:], w2d, ["cw2" + s], eng="pool")
            w.ld(b1s[:, si:si + 1], b1d, ["b1s"])
            for r in range(32):
                w.mm(B[1][:, si:si + 1], cw1[:, r, :], posb[:, r:r + 1], r == 0, r == 31, ["cw1", "posb"], ["B1"])
            w.tt(cpos[s][:], B[1][:, si:si + 1], b1s[:, si:si + 1], ALU.add, ["B1", "b1s"], ["cpos" + s])
            w.memset(cext[s][:], 0.0, ["cext" + s]); w.memset(hidT[s][:], 0.0, ["hidT" + s])
        w.memset(kcmpT[:], 0.0, ["kcmpT"]); w.memset(vcmp[:], 0.0, ["vcmp"])
        w.memset(S32[:], 0.0, ["S32"]); w.memset(S16v[:], 0.0, ["S16v0", "S16v1", "S16v2", "S16v3"])

        def load_cw1(s):
            w.ld(cw1[:], cmpw[s][1].rearrange("(r d) m -> d r m", d=128), ["cw1"], eng="pool")

        for I in range(NB if stage >= 1 else 0):
            t0 = I * 512
            for s4 in range(4):
                w.ld(xt[:], x[t0 + s4 * 128:t0 + (s4 + 1) * 128, :], ["xt"])
                w_ = P.op("dve", lambda e: e.bn_stats(out=stat[:, 0:6], in_=xt[:, 0:512]), ["xt"], ["stat"])
                P.op("dve", lambda e: e.bn_stats(out=stat[:, 6:12], in_=xt[:, 512:1024]), ["xt"], ["stat"])
                P.op("dve", lambda e: e.bn_aggr(out=stat[:, 12:14], in_=stat[:, 0:12].rearrange("p (a b) -> p a b", a=2)), ["stat"], ["stat"])
                w.act(stat[:, 14:15], stat[:, 13:14], AF.Ln, ["stat"], ["stat"], bias=1e-5)
                w.act(stat[:, 14:15], stat[:, 14:15], AF.Exp, ["stat"], ["stat"], scale=-0.5)
                w.stt(stat[:, 15:16], stat[:, 12:13], -1.0, stat[:, 14:15], ALU.mult, ALU.mult, ["stat"], ["stat"])
                w.act(xn[:, s4, :], xt[:], AF.Identity, ["xt", "stat"], ["xn"], bias=stat[:, 15:16], scale=stat[:, 14:15])
            for c in range(8):
                tb = TB[c % 2]
                for s4 in range(4):
                    w.tr(tb[:, s4 * 128:(s4 + 1) * 128], xn[:, s4, c * 128:(c + 1) * 128], identb[:], ["xn", "identb"], ["TB%d" % (c % 2)])
                w.ts(hT[:, c, :], tb[:, 0:512], scp1[:, c:c + 1], mod1[:, c:c + 1], ALU.mult, ALU.add, ["TB%d" % (c % 2), "scp1", "mod1"], ["hT"])

            dbg("hT", hT[:], [128, 8, 512], BF16, ["hT"])
            dbg("mod1", mod1[:], [128, 16], F32, ["mod1"])

            def projF(m, bank):
                rows = 12 if m == 20 else 128
                for kc in range(8):
                    w.mm(B[bank][0:rows, :], wFb[:, kc, m * 128:m * 128 + rows], hT[:, kc, :], kc == 0, kc == 7, ["wFb", "hT"], ["B%d" % bank])
                return rows

            for s4 in range(4):
                j = I * 4 + s4
                bk = s4 % 2
                for kc in range(8):
                    w.mm(B[bk][:, :], hT[:, kc, s4 * 128:(s4 + 1) * 128], wTb[:, kc, 0:512], kc == 0, False, ["hT", "wTb"], ["B%d" % bk])
                w.mm(B[bk][:, :], onesb[0:1, :], bTb[0:1, 0:512], False, True, ["onesb", "bTb"], ["B%d" % bk])
                w.cp(vtk[:, s4, :], B[bk][:, :], ["B%d" % bk], ["vtk"], eng="act")
                bk2 = 2 + s4 % 2
                for kc in range(8):
                    w.mm(B[bk2][:, 0:256], hT[:, kc, s4 * 128:(s4 + 1) * 128], wTb[:, kc, 512:768], kc == 0, False, ["hT", "wTb"], ["B%d" % bk2])
                w.mm(B[bk2][:, 0:256], onesb[0:1, :], bTb[0:1, 512:768], False, True, ["onesb", "bTb"], ["B%d" % bk2])
                w.cp(vs[:, j, :], B[bk2][:, 0:128], ["B%d" % bk2], ["vs"], eng="act")
                w.cp(vw[:, j % 8, :], B[bk2][:, 128:256], ["B%d" % bk2], ["vw"], eng="act")

            for h in range(4):
                projF(12 + h, h % 2)
                w.act(nqT[:, h, :], B[h % 2][:, :], AF.Identity, ["B%d" % (h % 2), "bFq"], ["nqT"], bias=bFq[:, h:h + 1], scale=SCALE)
            for (m, s) in ((16, "k"), (17, "v")):
                projF(m, 0)
                w.cp(cext[s][:, 0:16], cext[s][:, 512:528], ["cext" + s], ["cext" + s], eng="pool")
                w.act(cext[s][:, 16:528], B[0][:, :], AF.Identity, ["B0", "bFs"], ["cext" + s], bias=bFs[:, m:m + 1])
            projF(18, 1)
            w.act(ksT[:, t0:t0 + 512], B[1][:, :], AF.Identity, ["B1", "bFs"], ["ksT"], bias=bFs[:, 18:19])
            projF(19, 0)
            for s4 in range(4):
                w.act(kwT[:, (I * 4 + s4) % 8, :], B[0][:, s4 * 128:(s4 + 1) * 128], AF.Identity, ["B0", "bFs"], ["kwT"], bias=bFs[:, 19:20])
            projF(20, 1)
            w.act(gate[:, :], B[1][0:12, :], AF.Sigmoid, ["B1", "bFs"], ["gate"], bias=bFs[0:12, 20:21])

            for h in range(4 if stage >= 2 else 0):
                projF(h, 0)
                w.act(qT[:], B[0][:, :], AF.Identity, ["B0", "bFs"], ["qT"], bias=bFs[:, h:h + 1])
                projF(4 + h, 1)
                w.act(sg[:], B[1][:, :], AF.Sigmoid, ["B1", "bFs"], ["sg"], bias=bFs[:, 4 + h:5 + h])
                projF(8 + h, 0)
                w.act(gT[:], B[0][:, :], AF.Sigmoid, ["B0", "bFs"], ["gT"], bias=bFs[:, 8 + h:9 + h])
                w.ts(t1[:], sg[:], oml[:, h:h + 1], lb[:, h:h + 1], ALU.mult, ALU.add, ["sg", "oml", "lb"], ["t1"])
                w.act(t2[:], t1[:], AF.Ln, ["t1"], ["t2"])
                w.ts(t1[:], t1[:], -1.0, 1.0, ALU.mult, ALU.add, ["t1"], ["t1"])
                for s4 in range(4):
                    sl = slice(s4 * 128, (s4 + 1) * 128)
                    P.op("dve", lambda e, sl=sl: e.tensor_tensor_scan(out=t3[:, sl], data0=onesf[:, :], data1=t2[:, sl], initial=0.0,
                                                                      op0=ALU.mult, op1=ALU.add), ["t2", "onesf"], ["t3"])
                w.act(t2[:], t3[:], AF.Exp, ["t3"], ["t2"])
                w.act(t4[:], t3[:], AF.Exp, ["t3"], ["t4"], scale=-1.0)
                w.act(dec[:, :], t3[:, 127:512:128], AF.Exp, ["t3"], ["dec"])
                dbg("qT", qT[:], [128, 512], F32, ["qT"]); dbg("sg", sg[:], [128, 512], F32, ["sg"]); dbg("gT", gT[:], [128, 512], F32, ["gT"])
                dbg("b", t3[:], [128, 512], F32, ["t3"]); dbg("k", t1[:], [128, 512], F32, ["t1"]); dbg("vtk", vtk[:], [128, 4, 512], BF16, ["vtk"])
                dbg("dec", dec[:], [128, 4], F32, ["dec"])
                w.tt(qe[:], qT[:], t2[:], ALU.mult, ["qT", "t2"], ["qe"])
                w.tt(t4[:], t1[:], t4[:], ALU.mult, ["t1", "t4"], ["t4"])
                w.cp(kin[:], t4[:], ["t4"], ["kin"], eng="pool")
                for s4 in range(4):
                    sl = slice(s4 * 128, (s4 + 1) * 128)
                    w.ts(kendT[:, sl], t4[:, sl], dec[:, s4:s4 + 1], None, ALU.mult, ALU.bypass, ["t4", "dec"], ["kendT"])
                for s4 in range(4):
                    sl = slice(s4 * 128, (s4 + 1) * 128)
                    w.tr(TB[1][:, sl], kendT[:, sl], identb[:], ["kendT", "identb"], ["TB1"])
                w.cp(kend4[:], TB[1][:, 0:512], ["TB1"], ["kend4"], eng="act")
                for s4 in range(4):
                    sl = slice(s4 * 128, (s4 + 1) * 128)
                    w.mm(B[4][:, sl], kend4[:, sl], vtk[:, s4, h * 128:(h + 1) * 128], s4 == 0, s4 == 3, ["kend4", "vtk"], ["B4"])
                if I > 0:
                    w.cp(S16v[:, h, 0, :], S16v[:, h, 4, :], ["S16v%d" % h], ["S16v%d" % h], eng="act")
                for s4 in range(4):
                    sl = slice(s4 * 128, (s4 + 1) * 128)
                    w.stt(S32[:, h, :], S32[:, h, :], dec[:, s4:s4 + 1], B[4][:, sl], ALU.mult, ALU.add, ["S32", "dec", "B4"], ["S32"])
                    w.cp(S16v[:, h, s4 + 1, :], S32[:, h, :], ["S32"], ["S16v%d" % h], eng="act")
                for s4 in range(4):
                    sl = slice(s4 * 128, (s4 + 1) * 128)
                    w.mm(B[2][:, sl], kin[:, sl], qe[:, sl], s4 == 0, s4 == 3, ["kin", "qe"], ["B2"])
                for s4 in range(4):
                    sl = slice(s4 * 128, (s4 + 1) * 128)
                    w.tt(attm4[:, sl], B[2][:, sl], triU[:], ALU.mult, ["B2", "triU"], ["attm4"])
                for s4 in range(4):
                    sl = slice(s4 * 128, (s4 + 1) * 128)
                    w.mm(B[3][:, sl], vtk[:, s4, h * 128:(h + 1) * 128], attm4[:, sl], s4 == 0, False, ["vtk", "attm4"], ["B3"])
                    w.mm(B[3][:, sl], S16v[:, h, s4, :], qe[:, sl], False, s4 == 3, ["S16v%d" % h, "qe"], ["B3"])
                w.cp(oT[:], B[3][:, :], ["B3"], ["stg"], eng="act")
                dbg("stg", oT[:], [128, 512], F32, ["stg"])
                w.tt(osq[:], oT[:], oT[:], ALU.mult, ["stg"], ["stg"], eng="pool")
                w.mm(B[5][:, :], onesf[:], osq[:], True, True, ["onesf", "stg"], ["B5"])
                w.act(osq[:], B[5][:, :], AF.Ln, ["B5"], ["stg"], bias=1e-6, scale=1.0 / 128)
                w.act(osq[:], osq[:], AF.Exp, ["stg"], ["stg"], scale=-0.5)
                w.tt(oT[:], oT[:], osq[:], ALU.mult, ["stg", "stg"], ["stg"])
                w.stt(ohb[:], oT[:], nw[:, h:h + 1], gT[:], ALU.mult, ALU.mult, ["stg", "nw", "gT"], ["ohb"])
                w.st(ohT_o[g * 512 + h * 128:g * 512 + (h + 1) * 128, t0:t0 + 512], ohb[:], ["ohb"])

            if stage < 3:
                continue
            lo = 1 if I == 0 else 0
            c0, c1 = 32 * I - 1 + lo, 32 * I + 31
            for s in "kv":
                load_cw1(s)
                for r in range(32):
                    w.mm(B[0][:, 0:32], cw1[:, r, :], cext[s][:, r:r + 497:16], r == 0, r == 31, ["cw1", "cext" + s], ["B0"])
                w.act(hidT[s][:, c0:c1], B[0][:, lo:32], AF.Silu, ["B0", "cpos" + s], ["hidT" + s], bias=cpos[s][:, 0:1])
            w.mm(B[1][:, 0:c1 - c0], cw2["k"][:], hidT["k"][:, c0:c1], True, True, ["cw2k", "hidTk"], ["B1"])
            w.cp(kcmpT[:, c0:c1], B[1][:, 0:c1 - c0], ["B1"], ["kcmpT"], eng="act")
            for jt in sorted(set([max(c0, 0) // 128, (c1 - 1) // 128])):
                w.mm(B[1][:, 0:128], hidT["v"][:, jt * 128:(jt + 1) * 128], cw2["v"][:], True, True, ["hidTv", "cw2v"], ["B1"])
                w.cp(vcmp[:, jt, :], B[1][:, 0:128], ["B1"], ["vcmp"], eng="act")

            def run_units(h, units, want_U):
                nu = len(units)
                sbanks = [0, 1] if want_U else [0, 1, 4]
                nbk = len(sbanks)
                la = nbk - 1

                def emit_qk(u):
                    Kap, kreads, extras, Vap, vreads, ovap = units[u]
                    bk = sbanks[u % nbk]
                    bkey = "B%d" % bk
                    w.mm(B[bk][:, :], Kap, nqT[:, h, :], True, len(extras) == 0, kreads + ["nqT"], [bkey])
                    for ei, (cols, lT, rh, rds) in enumerate(extras):
                        w.mm(B[bk][:, cols], lT, rh, False, ei == len(extras) - 1, rds, [bkey])

                def emit_rest(u):
                    Kap, kreads, extras, Vap, vreads, ovap = units[u]
                    bk = sbanks[u % nbk]
                    bkey = "B%d" % bk
                    pt = PT[u % nbk]
                    pkey = "PT%d" % (u % nbk)
                    w.act(pt[:], B[bk][:, :], AF.Exp, [bkey, "c31"], [pkey], bias=c31[:, h:h + 1])
                    first, last = (u == 0), (u == nu - 1)
                    w.mm(B[2][:, :], Vap, pt[:], first, last, vreads + [pkey], ["B2"])
                    w.mm(B[3][:, :], onesb[:], pt[:], first, last, ["onesb", pkey], ["B3"])
                    if want_U:
                        for s4 in range(4):
                            sl = slice(s4 * 128, (s4 + 1) * 128)
                            w.mm(B[4][:, sl], pt[:, sl], ovap, first and s4 == 0, last and s4 == 3, [pkey, "ovm"], ["B4"])
                            w.mm(B[5][:, s4:s4 + 1], pt[:, sl], onesb[:, 0:1], first and s4 == 0, last and s4 == 3, [pkey, "onesb"], ["B5"])

                for u in range(min(la, nu)):
                    emit_qk(u)
                for u in range(nu):
                    if u + la < nu:
                        emit_qk(u + la)
                    emit_rest(u)

            def epilogue(h, br, first):
                if not first:
                    w.cp(t3[:], B[2][:, :], ["B2"], ["t3"], eng="act")
                w.ts(rZ[:], B[3][:, :], 1e-30, None, ALU.max, ALU.bypass, ["B3"], ["t1"])
                P.op("dve", lambda e: e.reciprocal(out=rZ[:], in_=rZ[:]), ["t1"], ["t1"])
                r = 3 * h + br
                w.mm(B[5][:, :], selrow[0:12, r * 128:(r + 1) * 128], gate[0:12, :], True, True, ["selrow", "gate"], ["B5"])
                w.tt(fac[:], rZ[:], B[5][:, :], ALU.mult, ["t1", "B5"], ["t2"])
                if first:
                    w.tt(oacc4[:, h, :], B[2][:, :], fac[:], ALU.mult, ["B2", "t2"], ["oacc%d" % h])
                else:
                    w.tt(rZ[:], t3[:], fac[:], ALU.mult, ["t3", "t2"], ["t1"])
                    w.tt(oacc4[:, h, :], oacc4[:, h, :], rZ[:], ALU.add, ["oacc%d" % h, "t1"], ["oacc%d" % h], eng="pool")

            def diag_extras(h, j, window):
                ex = []
                for s4 in range(4):
                    o = 4 * I + s4 - j
                    if o < 0:
                        kind = 3
                    elif o == 0:
                        kind = 0
                    elif o == 1:
                        kind = 1
                    elif window and o == 4:
                        kind = int(os.environ.get("X_K2", "2"))
                    elif window and o >= 5:
                        kind = 3
                    else:
                        continue
                    ex.append((slice(s4 * 128, (s4 + 1) * 128), identb[:], D4t[:, h, :] if kind == 2 else Dt[:, h, kind, :], ["identb", "Dt"]))
                return ex

            for h in range(4 if stage >= 4 else 0):
                units = []
                for jt in range(I // 4 + 1):
                    o = I - 4 * jt
                    ex = []
                    if o <= 4:
                        w.ld(stg[:, 0:512], biasC[h, o], ["stg"])
                        w.ld(stg2[:], maskC_d[o], ["t4"])
                        w.stt(cbt[jt % 2][:], stg[:, 0:512], c31[:, h:h + 1], stg2[:], ALU.subtract, ALU.add, ["stg", "t4", "c31"], ["cbt%d" % (jt % 2)])
                        ex.append((slice(0, 512), identb[:], cbt[jt % 2][:], ["identb", "cbt%d" % (jt % 2)]))
                    units.append((kcmpT[:, jt * 128:(jt + 1) * 128], ["kcmpT"], ex, vcmp[:, jt, :], ["vcmp"], ovm[:, jt, :]))
                run_units(h, units, True)
                w.ts(rzt[:, 0:4], B[5][:, 0:4], 1e-30, None, ALU.max, ALU.bypass, ["B5"], ["rzt"])
                P.op("dve", lambda e: e.reciprocal(out=rzt[:, 0:4], in_=rzt[:, 0:4]), ["rzt"], ["rzt"])
                for s4 in range(4):
                    sl = slice(s4 * 128, (s4 + 1) * 128)
                    if h == 0:
                        w.ts(impacc[:, sl], B[4][:, sl], rzt[:, s4:s4 + 1], None, ALU.mult, ALU.bypass, ["B4", "rzt"], ["t3"])
                    else:
                        w.stt(impacc[:, sl], B[4][:, sl], rzt[:, s4:s4 + 1], impacc[:, sl], ALU.mult, ALU.add, ["B4", "rzt", "t3"], ["t3"])
                epilogue(h, 0, True)

            for s4 in range(4 if stage >= 5 else 0):
                tq = 4 * I + s4
                off = 128 - 2 * tq
                sl = slice(s4 * 128, (s4 + 1) * 128)
                w.tt(sc[:], impacc[:, sl], selA[:, off:off + 128], ALU.mult, ["t3", "selA"], ["sc"])
                w.tt(sc[:], sc[:], selB[:, off:off + 128], ALU.add, ["sc", "selB"], ["sc"])
                w.memset(sc[:, 0:1], 1e4, ["sc"], eng="dve")
                P.op("dve", lambda e: e.max(out=m8[:, 0:8], in_=sc[:]), ["sc"], ["m8"])
                P.op("dve", lambda e: e.match_replace(out=scw[:], in_to_replace=m8[:, 0:8], in_values=sc[:], imm_value=-1e30), ["sc", "m8"], ["scw"])
                P.op("dve", lambda e: e.max(out=m8[:, 8:16], in_=scw[:]), ["scw"], ["m8"])
                w.ts(scw[:], sc[:], m8[:, 15:16], None, ALU.is_ge, ALU.bypass, ["sc", "m8"], ["scw"])
                w.ts(nsel[:], scw[:], 32768.0, -32768.0, ALU.mult, ALU.add, ["scw"], ["nsel"])
                for g4 in range(4):
                    w.tr(TB[0][0:32, g4 * 128:(g4 + 1) * 128], nsel[:, g4 * 32:(g4 + 1) * 32], identb[:], ["nsel", "identb"], ["TB0"])
                w.cp(nselT[0:32, :, sl], TB[0][0:32, 0:512].rearrange("p (g q) -> p g q", g=4), ["TB0"], ["nselT"], eng="act")

            for h in range(4 if stage >= 6 else 0):
                units = []
                for j in range(max(0, 4 * I - 4), 4 * I + 4 if os.environ.get("X_WIN", "1") == "1" else 0):
                    units.append((kwT[:, j % 8, :], ["kwT"], diag_extras(h, j, True) if os.environ.get("X_WEX", "1") == "1" else [], vw[:, j % 8, :], ["vw"], None))
                if units:
                    run_units(h, units, False)
                    epilogue(h, 2, False)
            for h in range(4 if stage >= 6 else 0):
                units = []
                for j in range(4 * I + 4):
                    ex = []
                    if os.environ.get("X_MASK", "1") == "1":
                        ex = [(slice(0, 512), H32[0:32, 128 * (j % 16):128 * (j % 16) + 128], nselT[0:32, j // 16, :], ["H32", "nselT"])]
                    if j >= 4 * I - 1 and os.environ.get("X_DIAG", "1") == "1":
                        ex += diag_extras(h, j, False)
                    units.append((ksT[:, j * 128:(j + 1) * 128], ["ksT"], ex, vs[:, j, :], ["vs"], None))
                run_units(h, units, False)
                epilogue(h, 1, False)
                w.cp(onb[:], oacc4[:, h, :], ["oacc%d" % h], ["ohb"], eng="act")
                w.st(onT_o[g * 512 + h * 128:g * 512 + (h + 1) * 128, t0:t0 + 512], onb[:], ["ohb"])


ALPHA_C = float(2.0 ** 0.25)


def host_inputs_p2(inp, b, s, TT):
    d = {}
    tsl = slice(s * TT, (s + 1) * TT)
    d["x"] = np.ascontiguousarray(inp["x"][b, tsl])
    d["cT"] = np.ascontiguousarray(inp["c"][b].reshape(8, 128).T)
    d["adaw"] = inp["ada_w"][0]
    ab = inp["ada_b"][0]
    d["adabT"] = np.ascontiguousarray(ab.reshape(48, 128).T)
    d["adabB"] = np.ascontiguousarray(np.broadcast_to(ab[None, :], (128, 6144)))
    w_in = inp["w_in"][0]
    d["wG"] = np.ascontiguousarray(w_in[:, 6680:8728])
    d["bG"] = np.ascontiguousarray(inp["b_in"][0][6680:8728].reshape(16, 128).T)
    d["wbh"] = inp["w_br_hg"][0]; d["wbn"] = inp["w_br_nsa"][0]; d["wout"] = inp["w_out"][0]
    for nm in ("ln1_g", "ln1_b", "ln2_g", "ln2_b"):
        d[nm] = np.ascontiguousarray(np.broadcast_to(inp[nm][0][None, :], (128, 1024)))
    d["wr"] = np.ascontiguousarray(np.concatenate([inp["router_grp_w"][0], inp["router_exp_w"][0]], axis=1))
    d["br"] = np.ascontiguousarray(np.broadcast_to(np.concatenate([inp["router_grp_b"][0], inp["router_exp_b"][0]])[None, :], (128, 36)))
    d["ew1"] = inp["exp_w1"][0]; d["ew3"] = inp["exp_w3"][0]; d["ew2"] = inp["exp_w2"][0]
    d["ident"] = np.eye(128, dtype=np.float32)
    return d


def emit_p2(nc, P, w, TT, ohT, onT, n_exp=32):
    NB = TT // 512
    def din(name, shape, dt=F32):
        return nc.dram_tensor("q_" + name, list(shape), dt, kind="ExternalInput").ap()

    x = din("x", [TT, D]); hsel_d = din("hsel", [128, 2])
    cT = din("cT", [128, 8]); adaw = din("adaw", [D, 6144]); adabT = din("adabT", [128, 48]); adabB = din("adabB", [128, 6144])
    wG = din("wG", [D, 2048]); bG = din("bG", [128, 16])
    wbh = din("wbh", [D, D]); wbn = din("wbn", [D, D]); wout = din("wout", [D, D])
    lnd = {nm: din(nm, [128, D]) for nm in ("ln1_g", "ln1_b", "ln2_g", "ln2_b")}
    wr = din("wr", [D, 36]); br = din("br", [128, 36])
    ew1 = din("ew1", [32, D, 512]); ew3 = din("ew3", [32, D, 512]); ew2 = din("ew2", [32, 512, D])
    ident_d = din("ident", [128, 128])
    out_o = nc.dram_tensor("out", [TT, D], F32, kind="ExternalOutput").ap()
    sb, ps = P.sb, P.ps
    hsel = sb("hsel", [128, 2], F32); xa = sb("xa", [128, 512], BF16); xb = sb("xb", [128, 512], BF16)
    big = sb("big", [128, 40960], BF16)
    wGb = big[:, 0:16384].rearrange("p (k c) -> p k c", k=8)
    wbhb = big[:, 16384:24576].rearrange("p (k c) -> p k c", k=8)
    wbnb = big[:, 24576:32768].rearrange("p (k c) -> p k c", k=8)
    woutb = big[:, 32768:40960].rearrange("p (k c) -> p k c", k=8)
    wrf = sb("wrf", [128, 8, 36], F32); brs = sb("brs", [128, 36], F32)
    bGs = sb("bGs", [128, 16], F32)
    identb = sb("identb", [128, 128], BF16); identf = sb("identf", [128, 128], F32); onesf = sb("onesf", [128, 128], F32)
    silc = sb("silc", [128, 8], F32); silcB = sb("silcB", [128, 8, 128], F32)
    modT = sb("modT", [128, 48], F32); scp1 = sb("scp1", [128, 8], F32); scp2 = sb("scp2", [128, 8], F32)
    g1b = sb("g1b", [128, D], F32); g2b = sb("g2b", [128, D], F32)
    lnt = {nm: sb("t_" + nm, [128, D], F32) for nm in lnd}
    stg = sb("stg", [128, 1024], F32)
    xt = sb("xt", [128, D], F32); xr = xt; xn = sb("xn", [128, 4, D], BF16); stat = sb("stat", [128, 16], F32)
    hT = sb("hT", [128, 8, 512], BF16); h2T = hT; h2f = stg
    h2T32 = sb("h2T32", [128, 8, 128], F32)
    oh = sb("oh", [128, 8, 512], BF16); on = sb("on", [128, 8, 512], BF16)
    sgh = sb("sgh", [128, 512], F32); sgn = sb("sgn", [128, 512], F32)
    mT = sb("mT", [128, 8, 512], BF16)
    zt = sb("zt", [128, D], F32); x1 = sb("x1", [128, 4, D], F32)
    lg = sb("lg", [128, 36], F32); rt = sb("rt", [128, 48], F32); m8 = sb("m8", [128, 8], F32)
    elm = sb("elm", [128, 32], F32); eq = sb("eq", [128, 32], F32); C = sb("C", [128, 4, 32], F32)
    ew1b = [big[:, i * 12288:i * 12288 + 4096].rearrange("p (k c) -> p k c", k=8) for i in range(2)]
    ew3b = [big[:, i * 12288 + 4096:i * 12288 + 8192].rearrange("p (k c) -> p k c", k=8) for i in range(2)]
    ew2b = [big[:, i * 12288 + 8192:i * 12288 + 12288].rearrange("p (k c) -> p k c", k=4) for i in range(2)]
    s1 = sgh; aT = mT
    yacc = sb("yacc", [128, 4, D], F32)
    B = [ps("B%d" % i, [128, 512], F32) for i in range(6)]
    TB = [ps("TB%d" % i, [128, 1024], BF16) for i in range(2)]

    w.memset(onesf[:], 1.0, ["onesf"])
    w.ld(identb[:], ident_d, ["identb"], eng="pool"); w.ld(identf[:], ident_d, ["identf"])
    w.ld(hsel[:], hsel_d, ["hsel"]); w.ld(silc[:], cT, ["silc"]); w.ld(modT[:], adabT, ["adabT_s"]); w.ld(bGs[:], bG, ["bGs"]); w.ld(brs[:], br, ["brs"])
    for nm in lnd:
        w.ld(lnt[nm][:], lnd[nm], ["t_" + nm])
    for kc in range(8):
        rs = slice(kc * 128, (kc + 1) * 128)
        w.ld(wrf[:, kc, :], wr[rs, :], ["wrf"])
    w.act(silc[:], silc[:], AF.Silu, ["silc"], ["silc"])
    for kc in range(8):
        w.ts(silcB[:, kc, :], onesf[:], silc[:, kc:kc + 1], None, ALU.mult, ALU.bypass, ["onesf", "silc"], ["silcB"])
    for m in range(48):
        w.ld(stg[:, 0:1024].rearrange("p (k c) -> p k c", k=8), adaw[:, m * 128:(m + 1) * 128].rearrange("(k p) c -> p k c", p=128), ["stg"])
        for kc in range(8):
            w.mm(B[0][:, m:m + 1], stg[:, kc * 128:(kc + 1) * 128], silc[:, kc:kc + 1], kc == 0, kc == 7, ["stg", "silc"], ["B0"])
    w.tt(modT[:], modT[:], B[0][:, 0:48], ALU.add, ["adabT_s", "B0"], ["modT"])
    w.ts(scp1[:], modT[:, 8:16], 1.0, None, ALU.add, ALU.bypass, ["modT"], ["scp1"])
    w.ts(scp2[:], modT[:, 32:40], 1.0, None, ALU.add, ALU.bypass, ["modT"], ["scp2"])
    for (dst, c0, nm) in ((g1b, 2048, "g1b"), (g2b, 5120, "g2b")):
        w.ld(dst[:, :], adabB[:, c0:c0 + 1024], [nm])
        for pc in range(8):
            cs = slice(c0 + pc * 128, c0 + pc * 128 + 128)
            w.ld(stg[:, 0:1024].rearrange("p (k c) -> p k c", k=8), adaw[:, cs].rearrange("(k p) c -> p k c", p=128), ["stg"])
            for kc in range(8):
                w.mm(B[1][:, 0:128], silcB[:, kc, :], stg[:, kc * 128:(kc + 1) * 128], kc == 0, kc == 7, ["silcB", "stg"], ["B1"])
            w.stt(dst[:, pc * 128:(pc + 1) * 128], dst[:, pc * 128:(pc + 1) * 128], 1.0, B[1][:, 0:128], ALU.add, ALU.add, [nm, "B1"], [nm])

    def layer_norm_stats(src_ap, rd):
        P.op("dve", lambda e: e.bn_stats(out=stat[:, 0:6], in_=src_ap[:, 0:512]), rd, ["stat"])
        P.op("dve", lambda e: e.bn_stats(out=stat[:, 6:12], in_=src_ap[:, 512:1024]), rd, ["stat"])
        P.op("dve", lambda e: e.bn_aggr(out=stat[:, 12:14], in_=stat[:, 0:12].rearrange("p (a b) -> p a b", a=2)), ["stat"], ["stat"])
        w.act(stat[:, 14:15], stat[:, 13:14], AF.Ln, ["stat"], ["stat"], bias=1e-5)
        w.act(stat[:, 14:15], stat[:, 14:15], AF.Exp, ["stat"], ["stat"], scale=-0.5)
        w.stt(stat[:, 15:16], stat[:, 12:13], -1.0, stat[:, 14:15], ALU.mult, ALU.mult, ["stat"], ["stat"])

    def load_expert(e, bi):
        for kc in range(8):
            rs = slice(kc * 128, (kc + 1) * 128)
            w.ld(ew1b[bi][:, kc, :], ew1[e, rs, :], ["E%d_1_%d" % (bi, kc)], eng="pool", reads=["P1DONE"])
            w.ld(ew3b[bi][:, kc, :], ew3[e, rs, :], ["E%d_3_%d" % (bi, kc)], eng="pool", reads=["P1DONE"])
        for kc in range(4):
            w.ld(ew2b[bi][:, kc, :], ew2[e, kc * 128:(kc + 1) * 128, :], ["E%d_2_%d" % (bi, kc)], eng="pool", reads=["P1DONE"])

    def load_weights():
        for kc in range(8):
            rs = slice(kc * 128, (kc + 1) * 128)
            w.ld(wGb[:, kc, :], wG[rs, :], ["WG%d" % kc], eng="pool", reads=["MOEDONE"]); w.ld(wbhb[:, kc, :], wbh[rs, :], ["WH%d" % kc], eng="pool", reads=["MOEDONE"])
            w.ld(wbnb[:, kc, :], wbn[rs, :], ["WN%d" % kc], eng="pool", reads=["MOEDONE"]); w.ld(woutb[:, kc, :], wout[rs, :], ["WO%d" % kc], eng="pool", reads=["MOEDONE"])

    for J in range(NB):
        t0 = J * 512
        load_weights()
        for (dst, src, nm) in ((oh, ohT, "oh"), (on, onT, "on")):
            for k in range(8):
                w.ld(xa[:], src[k * 128:(k + 1) * 128, t0:t0 + 512], ["xa"])
                w.ld(xb[:], src[k * 128:(k + 1) * 128, TT + t0:TT + t0 + 512], ["xb"])
                w.ts(dst[:, k, :], xa[:], hsel[:, 0:1], None, ALU.mult, ALU.bypass, ["xa", "hsel"], [nm])
                w.stt(dst[:, k, :], xb[:], hsel[:, 1:2], dst[:, k, :], ALU.mult, ALU.add, ["xb", "hsel", nm], [nm])
        for s4 in range(4):
            w.ld(xt[:], x[t0 + s4 * 128:t0 + (s4 + 1) * 128, :], ["xt"])
            layer_norm_stats(xt[:, :], ["xt"])
            w.act(xn[:, s4, :], xt[:], AF.Identity, ["xt", "stat"], ["xn"], bias=stat[:, 15:16], scale=stat[:, 14:15])
        for c in range(8):
            tb = TB[c % 2]
            for s4 in range(4):
                w.tr(tb[:, s4 * 128:(s4 + 1) * 128], xn[:, s4, c * 128:(c + 1) * 128], identb[:], ["xn", "identb"], ["TB%d" % (c % 2)])
            w.ts(hT[:, c, :], tb[:, 0:512], scp1[:, c:c + 1], modT[:, c:c + 1], ALU.mult, ALU.add, ["TB%d" % (c % 2), "scp1", "modT"], ["hT"])
        for c in range(8):
            cs = slice(c * 128, (c + 1) * 128)
            for kc in range(8):
                w.mm(B[0][:, :], wGb[:, kc, cs], hT[:, kc, :], kc == 0, kc == 7, ["WG%d" % kc, "hT"], ["B0"])
            w.act(sgh[:], B[0][:, :], AF.Sigmoid, ["B0", "bGs"], ["sgh"], bias=bGs[:, c:c + 1])
            for kc in range(8):
                w.mm(B[1][:, :], wGb[:, kc, 1024 + c * 128:1024 + (c + 1) * 128], hT[:, kc, :], kc == 0, kc == 7, ["WG%d" % kc, "hT"], ["B1"])
            w.act(sgn[:], B[1][:, :], AF.Sigmoid, ["B1", "bGs"], ["sgn"], bias=bGs[:, 8 + c:9 + c])
            for kc in range(8):
                w.mm(B[2][:, :], wbhb[:, kc, cs], oh[:, kc, :], kc == 0, kc == 7, ["WH%d" % kc, "oh"], ["B2"])
            for kc in range(8):
                w.mm(B[3][:, :], wbnb[:, kc, cs], on[:, kc, :], kc == 0, kc == 7, ["WN%d" % kc, "on"], ["B3"])
            w.tt(sgh[:], sgh[:], B[2][:, :], ALU.mult, ["sgh", "B2"], ["sgh"])
            w.tt(sgn[:], sgn[:], B[3][:, :], ALU.mult, ["sgn", "B3"], ["sgn"])
            w.tt(mT[:, c, :], sgh[:], sgn[:], ALU.add, ["sgh", "sgn"], ["mT"], eng="pool")
        for s4 in range(4):
            tsl = slice(s4 * 128, (s4 + 1) * 128)
            w.ld(xr[:], x[t0 + s4 * 128:t0 + (s4 + 1) * 128, :], ["xt"])
            for hf in range(2):
                fs = slice(hf * 512, (hf + 1) * 512)
                for kc in range(8):
                    w.mm(B[4][:, :], mT[:, kc, tsl], woutb[:, kc, fs], kc == 0, kc == 7, ["mT", "WO%d" % kc], ["B4"])
                w.tt(zt[:, fs], B[4][:, :], g1b[:, fs], ALU.mult, ["B4", "g1b"], ["zt"])
                w.stt(zt[:, fs], xr[:, fs], ALPHA_C, zt[:, fs], ALU.mult, ALU.add, ["xt", "zt"], ["zt"])
            layer_norm_stats(zt[:, :], ["zt"])
            w.act(zt[:], zt[:], AF.Identity, ["zt", "stat"], ["zt"], bias=stat[:, 15:16], scale=stat[:, 14:15])
            w.tt(zt[:], zt[:], lnt["ln1_g"][:], ALU.mult, ["zt", "t_ln1_g"], ["zt"])
            w.tt(x1[:, s4, :], zt[:], lnt["ln1_b"][:], ALU.add, ["zt", "t_ln1_b"], ["x1", "P1DONE"] if s4 == 3 else ["x1"])
            layer_norm_stats(x1[:, s4, :], ["x1"])
            w.act(h2f[:], x1[:, s4, :], AF.Identity, ["x1", "stat"], ["stg"], bias=stat[:, 15:16], scale=stat[:, 14:15])
            w.cp(xn[:, s4, :], h2f[:], ["stg"], ["xn"], eng="pool")
            for c in range(8):
                w.tr(B[5][:, 0:128], h2f[:, c * 128:(c + 1) * 128], identf[:], ["stg", "identf"], ["B5"])
                w.ts(h2T32[:, c, :], B[5][:, 0:128], scp2[:, c:c + 1], modT[:, 24 + c:25 + c], ALU.mult, ALU.add, ["B5", "scp2", "modT"], ["h2T32"])
            for kc in range(8):
                w.mm(B[5][:, 128:164], h2T32[:, kc, :], wrf[:, kc, :], kc == 0, kc == 7, ["h2T32", "wrf"], ["B5"])
            w.tt(lg[:], B[5][:, 128:164], brs[:], ALU.add, ["B5", "brs"], ["lg"])
            P.op("dve", lambda e: e.tensor_reduce(out=rt[:, 0:1], in_=lg[:, 0:4], axis=AX.X, op=ALU.max), ["lg"], ["rt"])
            w.ts(rt[:, 1:2], rt[:, 0:1], -1.0, None, ALU.mult, ALU.bypass, ["rt"], ["rt"])
            w.act(rt[:, 4:8], lg[:, 0:4], AF.Exp, ["lg", "rt"], ["rt"], bias=rt[:, 1:2])
            P.op("dve", lambda e: e.tensor_reduce(out=rt[:, 2:3], in_=rt[:, 4:8], axis=AX.X, op=ALU.add), ["rt"], ["rt"])
            P.op("dve", lambda e: e.reciprocal(out=rt[:, 3:4], in_=rt[:, 2:3]), ["rt"], ["rt"])
            w.ts(rt[:, 8:12], lg[:, 0:4], rt[:, 0:1], None, ALU.is_ge, ALU.bypass, ["lg", "rt"], ["rt"])
            w.ts(rt[:, 8:12], rt[:, 8:12], 1e9, -1e9, ALU.mult, ALU.add, ["rt"], ["rt"])
            for g in range(4):
                w.ts(elm[:, 8 * g:8 * g + 8], lg[:, 4 + 8 * g:12 + 8 * g], rt[:, 8 + g:9 + g], None, ALU.add, ALU.bypass, ["lg", "rt"], ["elm"])
            P.op("dve", lambda e: e.max(out=m8[:, 0:8], in_=elm[:]), ["elm"], ["m8"])
            w.tt(rt[:, 12:13], m8[:, 1:2], m8[:, 0:1], ALU.subtract, ["m8"], ["rt"])
            w.act(rt[:, 12:13], rt[:, 12:13], AF.Exp, ["rt"], ["rt"])
            w.ts(rt[:, 12:13], rt[:, 12:13], 1.0, None, ALU.add, ALU.bypass, ["rt"], ["rt"])
            P.op("dve", lambda e: e.reciprocal(out=rt[:, 13:14], in_=rt[:, 12:13]), ["rt"], ["rt"])
            w.ts(rt[:, 14:15], rt[:, 13:14], -1.0, 1.0, ALU.mult, ALU.add, ["rt"], ["rt"])
            w.tt(rt[:, 13:14], rt[:, 13:14], rt[:, 3:4], ALU.mult, ["rt"], ["rt"])
            w.tt(rt[:, 14:15], rt[:, 14:15], rt[:, 3:4], ALU.mult, ["rt"], ["rt"])
            w.ts(eq[:], elm[:], m8[:, 0:1], rt[:, 13:14], ALU.is_equal, ALU.mult, ["elm", "m8", "rt"], ["eq"])
            w.ts(C[:, s4, :], elm[:], m8[:, 1:2], rt[:, 14:15], ALU.is_equal, ALU.mult, ["elm", "m8", "rt"], ["C"])
            w.tt(C[:, s4, :], C[:, s4, :], eq[:], ALU.add, ["C", "eq"], ["C"])
        for c in range(8):
            tb = TB[c % 2]
            for s4 in range(4):
                w.tr(tb[:, s4 * 128:(s4 + 1) * 128], xn[:, s4, c * 128:(c + 1) * 128], identb[:], ["xn", "identb"], ["TB%d" % (c % 2)])
            w.ts(h2T[:, c, :], tb[:, 0:512], scp2[:, c:c + 1], modT[:, 24 + c:25 + c], ALU.mult, ALU.add, ["TB%d" % (c % 2), "scp2", "modT"], ["hT"])
        load_expert(0, 0)
        for e in range(n_exp):
            bi = e % 2
            if e + 1 < n_exp:
                load_expert(e + 1, 1 - bi)
            for hc in range(4):
                hs = slice(hc * 128, (hc + 1) * 128)
                for kc in range(8):
                    w.mm(B[0][:, :], ew1b[bi][:, kc, hs], h2T[:, kc, :], kc == 0, kc == 7, ["E%d_1_%d" % (bi, kc), "hT"], ["B0"])
                for kc in range(8):
                    w.mm(B[1][:, :], ew3b[bi][:, kc, hs], h2T[:, kc, :], kc == 0, kc == 7, ["E%d_3_%d" % (bi, kc), "hT"], ["B1"])
                w.act(s1[:], B[0][:, :], AF.Silu, ["B0"], ["sgh"])
                w.tt(aT[:, hc, :], s1[:], B[1][:, :], ALU.mult, ["sgh", "B1"], ["mT"])
            for s4 in range(4):
                tsl = slice(s4 * 128, (s4 + 1) * 128)
                for hf in range(2):
                    fs = slice(hf * 512, (hf + 1) * 512)
                    bk = 2 + (s4 * 2 + hf) % 2
                    for kc in range(4):
                        w.mm(B[bk][:, :], aT[:, kc, tsl], ew2b[bi][:, kc, fs], kc == 0, kc == 3, ["mT", "E%d_2_%d" % (bi, kc)], ["B%d" % bk])
                    if e == 0:
                        w.ts(yacc[:, s4, fs], B[bk][:, :], C[:, s4, e:e + 1], None, ALU.mult, ALU.bypass, ["B%d" % bk, "C"], ["yacc"])
                    else:
                        w.stt(yacc[:, s4, fs], B[bk][:, :], C[:, s4, e:e + 1], yacc[:, s4, fs], ALU.mult, ALU.add, ["B%d" % bk, "C", "yacc"],
                              ["yacc", "MOEDONE"] if (e == n_exp - 1 and s4 == 3 and hf == 1) else ["yacc"])
        for s4 in range(4):
            w.tt(zt[:], yacc[:, s4, :], g2b[:], ALU.mult, ["yacc", "g2b"], ["zt"])
            w.stt(zt[:], x1[:, s4, :], ALPHA_C, zt[:], ALU.mult, ALU.add, ["x1", "zt"], ["zt"])
            layer_norm_stats(zt[:, :], ["zt"])
            w.act(zt[:], zt[:], AF.Identity, ["zt", "stat"], ["zt"], bias=stat[:, 15:16], scale=stat[:, 14:15])
            w.tt(zt[:], zt[:], lnt["ln2_g"][:], ALU.mult, ["zt", "t_ln2_g"], ["zt"])
            w.tt(zt[:], zt[:], lnt["ln2_b"][:], ALU.add, ["zt", "t_ln2_b"], ["zt"])
            w.st(out_o[t0 + s4 * 128:t0 + (s4 + 1) * 128, :], zt[:], ["zt"], is_out=True)


def build_fused(T, n_exp=32):
    nc = bass.Bass("TRN2", target_bir_lowering=False)
    P = Prog(nc)
    w = W(P)
    ohT_x = nc.dram_tensor("xch_oh", [D, T], BF16, kind="Internal").ap()
    onT_x = nc.dram_tensor("xch_on", [D, T], BF16, kind="Internal").ap()
    P.pfx = "a_"
    emit_p1(nc, P, w, T, ohT_x, onT_x)
    P.barrier()
    P.release_tensors()
    P.pfx = "b_"
    emit_p2(nc, P, w, T // 2, ohT_x, onT_x, n_exp)
    P.finish()
    return nc


def host_inputs_fused(inp, b, s, T):
    d = {}
    for g in range(2):
        dg = host_inputs_p1(inp, b, g, T)
        for k, v in dg.items():
            if k in PER_GROUP:
                d["g%d_%s" % (g, k)] = v
            elif g == 0:
                d[k] = v
    d2 = host_inputs_p2(inp, b, s, T // 2)
    for k, v in d2.items():
        d["q_" + k] = v
    hs = np.zeros((128, 2), np.float32)
    hs[:, s] = 1.0
    d["q_hsel"] = hs
    return d


def kernel(**inputs):
    inp = {k: np.asarray(v) for k, v in inputs.items()}
    T = inp["x"].shape[1]
    nb = inp["x"].shape[0]
    ncores = 2 * nb
    nc = build_fused(T)
    maps = [host_inputs_fused(inp, c // 2, c % 2, T) for c in range(ncores)]
    r = run_bass_kernel_spmd(nc, maps, core_ids=list(range(ncores)))
    out = np.stack([np.concatenate([np.asarray(r.results[2 * b]["out"]), np.asarray(r.results[2 * b + 1]["out"])], axis=0) for b in range(nb)])
    return out.astype(np.float32)
```
